# Optimizing a Trainium2 kernel written in Bass

```python
import jax, jax.numpy as jnp
from jax import lax
import numpy as np

D_MODEL = 1024
BATCH = 8
SEQ = 4096
DEPTH = 2

N_MEM = 256
POOL_WINDOWS = (2, 4, 8, 16)
POOL_WIDTH = 512
POOL_GROUP = POOL_WIDTH // len(POOL_WINDOWS)
FOX_HEADS = 8
FOX_HEAD_DIM = 64
FOX_WIDTH = FOX_HEADS * FOX_HEAD_DIM
MEM_HEADS = 4
MEM_HEAD_DIM = 128
MEM_WIDTH = MEM_HEADS * MEM_HEAD_DIM
N_BRANCH = 3
Q_BLOCK = 128
D_FF = 2816
N_EXPERTS = 8
TOP_K = 2
D_EXPERT = 3584
N_DENSE = (DEPTH + 1) // 2
N_MOE = DEPTH // 2
EPS = 1e-6

OFF_POOL = 0
OFF_Q = OFF_POOL + POOL_WIDTH
OFF_K = OFF_Q + FOX_WIDTH
OFF_V = OFF_K + FOX_WIDTH
OFF_F = OFF_V + FOX_WIDTH
OFF_MQ = OFF_F + FOX_HEADS
OFF_G = OFF_MQ + MEM_WIDTH
N_IN = OFF_G + N_BRANCH * D_MODEL

kernel_name = "hybrid_pool_fox_memxattn_moe_block"


def rms_norm(x, g):
    xf = x.astype(jnp.float32)
    y = xf * lax.rsqrt(jnp.mean(xf * xf, axis=-1, keepdims=True) + EPS)
    return (y * g.astype(jnp.float32)).astype(x.dtype)


def pool_mixer(u, pool_w, pool_scale):
    B, S, _ = u.shape
    G = len(POOL_WINDOWS)
    uf = u.astype(jnp.float32).reshape(B, S, G, POOL_GROUP)
    cpad = jnp.pad(jnp.cumsum(uf, axis=1), ((0, 0), (1, 0), (0, 0), (0, 0)))
    pos = jnp.arange(S)
    outs = []
    for g, w in enumerate(POOL_WINDOWS):
        c = cpad[:, :, g]
        lag = jnp.pad(c, ((0, 0), (w, 0), (0, 0)))[:, 1:S + 1]
        cnt = jnp.minimum(pos + 1, w).astype(jnp.float32)[None, :, None]
        outs.append((c[:, 1:] - lag) / cnt - uf[:, :, g])
    d = jnp.stack(outs, axis=2)
    y = jnp.einsum('bsgc,gcd->bsgd', d, pool_w.astype(jnp.float32))
    return (y.reshape(B, S, POOL_WIDTH) * pool_scale.astype(jnp.float32)).astype(u.dtype)


def head_rms(x, g):
    return rms_norm(x, g)


def fox_attention(q, k, v, f_logit, q_g, k_g):
    B, S, _ = q.shape
    H, dh = FOX_HEADS, FOX_HEAD_DIM
    q = head_rms(q.reshape(B, S, H, dh), q_g)
    k = head_rms(k.reshape(B, S, H, dh), k_g)
    v = v.reshape(B, S, H, dh)
    logf = jax.nn.log_sigmoid(f_logit.astype(jnp.float32))
    c = jnp.cumsum(logf, axis=1).transpose(0, 2, 1)
    nb = S // Q_BLOCK
    qb = q.reshape(B, nb, Q_BLOCK, H, dh).transpose(1, 0, 2, 3, 4)
    cb = c.reshape(B, H, nb, Q_BLOCK).transpose(2, 0, 1, 3)
    kpos = jnp.arange(S)
    scale = dh ** -0.5

    def block(args):
        qi, ci, i = args
        s = jnp.einsum('bqhd,bkhd->bhqk', qi, k, preferred_element_type=jnp.float32) * scale
        s = s + (ci[..., :, None] - c[..., None, :])
        qpos = i * Q_BLOCK + jnp.arange(Q_BLOCK)
        s = jnp.where(kpos[None, :] <= qpos[:, None], s, -jnp.inf)
        p = jax.nn.softmax(s, axis=-1)
        return jnp.einsum('bhqk,bkhd->bqhd', p.astype(v.dtype), v)

    o = lax.map(block, (qb, cb, jnp.arange(nb)))
    return o.transpose(1, 0, 2, 3, 4).reshape(B, S, FOX_WIDTH)


def mem_attention(qm, mem_n, w_kv, q_g, k_g):
    B, S, _ = qm.shape
    M = mem_n.shape[1]
    H, dh = MEM_HEADS, MEM_HEAD_DIM
    kv = mem_n @ w_kv
    k = head_rms(kv[..., :MEM_WIDTH].reshape(B, M, H, dh), k_g)
    v = kv[..., MEM_WIDTH:].reshape(B, M, H, dh)
    q = head_rms(qm.reshape(B, S, H, dh), q_g)
    s = jnp.einsum('bshd,bmhd->bhsm', q, k, preferred_element_type=jnp.float32) * (dh ** -0.5)
    p = jax.nn.softmax(s, axis=-1)
    o = jnp.einsum('bhsm,bmhd->bshd', p.astype(v.dtype), v)
    return o.reshape(B, S, MEM_WIDTH)


def swiglu(h, w_gu, w_d):
    gu = h @ w_gu
    f = w_d.shape[0]
    return (jax.nn.silu(gu[..., :f]) * gu[..., f:]) @ w_d


def moe_ffn(h, w_r, b_r, w_gu, w_d):
    B, S, D = h.shape
    t = h.reshape(B * S, D)
    logits = (t @ w_r).astype(jnp.float32) + b_r.astype(jnp.float32)
    top_v, top_i = lax.top_k(logits, TOP_K)
    wts = jax.nn.softmax(top_v, axis=-1)
    combine = jnp.sum(jax.nn.one_hot(top_i, N_EXPERTS, dtype=jnp.float32) * wts[..., None], axis=1)
    y = jnp.zeros((B * S, D), jnp.float32)
    for e in range(N_EXPERTS):
        y = y + combine[:, e:e + 1] * swiglu(t, w_gu[e], w_d[e]).astype(jnp.float32)
    return y.reshape(B, S, D).astype(h.dtype)


def setup_inputs(seed: int = 0) -> dict:
    key = jax.random.key(seed)
    ks = jax.random.split(key, 24)
    D = D_MODEL
    nrm = lambda k, shape, fan: jax.random.normal(k, shape, jnp.float32) * (fan ** -0.5)
    gain = lambda k, shape: 1.0 + 0.02 * jax.random.normal(k, shape, jnp.float32)
    return {
        "x": jax.random.normal(ks[0], (BATCH, SEQ, D), jnp.float32),
        "mem": jax.random.normal(ks[1], (BATCH, N_MEM, D), jnp.float32),
        "mix_norm_g": gain(ks[2], (DEPTH, D)),
        "w_in": nrm(ks[3], (DEPTH, D, N_IN), D),
        "b_forget": jax.random.uniform(ks[4], (DEPTH, FOX_HEADS), jnp.float32, 1.0, 5.0),
        "fox_q_g": gain(ks[5], (DEPTH, FOX_HEAD_DIM)),
        "fox_k_g": gain(ks[6], (DEPTH, FOX_HEAD_DIM)),
        "pool_w": nrm(ks[7], (DEPTH, len(POOL_WINDOWS), POOL_GROUP, POOL_GROUP), POOL_GROUP),
        "pool_scale": gain(ks[8], (DEPTH, POOL_WIDTH)),
        "mem_norm_g": gain(ks[9], (DEPTH, D)),
        "w_mem_kv": nrm(ks[10], (DEPTH, D, 2 * MEM_WIDTH), D),
        "mem_q_g": gain(ks[11], (DEPTH, MEM_HEAD_DIM)),
        "mem_k_g": gain(ks[12], (DEPTH, MEM_HEAD_DIM)),
        "w_pool_br": nrm(ks[13], (DEPTH, POOL_WIDTH, D), POOL_WIDTH),
        "w_fox_br": nrm(ks[14], (DEPTH, FOX_WIDTH, D), FOX_WIDTH),
        "w_mem_br": nrm(ks[15], (DEPTH, MEM_WIDTH, D), MEM_WIDTH),
        "w_out": 0.5 * nrm(ks[16], (DEPTH, D, D), D),
        "ffn_norm_g": gain(ks[17], (DEPTH, D)),
        "w_ffn_gu": nrm(ks[18], (N_DENSE, D, 2 * D_FF), D),
        "w_ffn_down": 0.5 * nrm(ks[19], (N_DENSE, D_FF, D), D_FF),
        "w_router": nrm(ks[20], (N_MOE, D, N_EXPERTS), D),
        "b_router": 0.01 * jax.random.normal(ks[21], (N_MOE, N_EXPERTS), jnp.float32),
        "w_exp_gu": nrm(ks[22], (N_MOE, N_EXPERTS, D, 2 * D_EXPERT), D),
        "w_exp_down": 0.5 * nrm(ks[23], (N_MOE, N_EXPERTS, D_EXPERT, D), D_EXPERT),
    }


def reference(x, mem, mix_norm_g, w_in, b_forget, fox_q_g, fox_k_g, pool_w, pool_scale,
              mem_norm_g, w_mem_kv, mem_q_g, mem_k_g, w_pool_br, w_fox_br, w_mem_br, w_out,
              ffn_norm_g, w_ffn_gu, w_ffn_down, w_router, b_router, w_exp_gu, w_exp_down):
    B, S, D = x.shape
    for layer in range(DEPTH):
        h = rms_norm(x, mix_norm_g[layer])
        z = h @ w_in[layer]
        pool_o = pool_mixer(z[..., OFF_POOL:OFF_Q], pool_w[layer], pool_scale[layer])
        fox_o = fox_attention(z[..., OFF_Q:OFF_K], z[..., OFF_K:OFF_V], z[..., OFF_V:OFF_F],
                              z[..., OFF_F:OFF_MQ] + b_forget[layer].astype(z.dtype),
                              fox_q_g[layer], fox_k_g[layer])
        mem_n = rms_norm(mem, mem_norm_g[layer])
        mem_o = mem_attention(z[..., OFF_MQ:OFF_G], mem_n, w_mem_kv[layer],
                              mem_q_g[layer], mem_k_g[layer])
        gates = jax.nn.sigmoid(z[..., OFF_G:].astype(jnp.float32)).reshape(B, S, N_BRANCH, D)
        merged = (gates[:, :, 0] * (pool_o @ w_pool_br[layer]).astype(jnp.float32)
                  + gates[:, :, 1] * (fox_o @ w_fox_br[layer]).astype(jnp.float32)
                  + gates[:, :, 2] * (mem_o @ w_mem_br[layer]).astype(jnp.float32))
        x = x + merged.astype(x.dtype) @ w_out[layer]
        h = rms_norm(x, ffn_norm_g[layer])
        if layer % 2 == 0:
            y = swiglu(h, w_ffn_gu[layer // 2], w_ffn_down[layer // 2])
        else:
            y = moe_ffn(h, w_router[layer // 2], b_router[layer // 2],
                        w_exp_gu[layer // 2], w_exp_down[layer // 2])
        x = x + y.astype(x.dtype)
    return x
```

```python
import numpy as np
import concourse.bass as bass
import concourse.mybir as mybir
from concourse.bass_utils import run_bass_kernel_spmd

F32 = mybir.dt.float32
BF16 = mybir.dt.bfloat16
I32 = mybir.dt.int32
AF = mybir.ActivationFunctionType
ALU = mybir.AluOpType
AX = mybir.AxisListType

D = 1024
S = 4096
DEPTH = 2
NMEM = 256
NIN = 5640
OFF_POOL, OFF_Q, OFF_K, OFF_V, OFF_F, OFF_MQ, OFF_G = 0, 512, 1024, 1536, 2048, 2056, 2568
DFF = 2816
NEXP = 8
DEXP = 3584
EPS = 1e-6
TT = 512
NT = S // TT
FT = 1024
SLOT_COLS = 4096
NSLOT = 3
CAP = 1280
NSL = NEXP * CAP
SEGS = [(0, 512), (512, 512), (1024, 256)]
BIG = 1.0e6


class Buf:
    __slots__ = ("name", "writer", "readers", "dsems")

    def __init__(self, name):
        self.name = name
        self.writer = None
        self.readers = {}
        self.dsems = {}


class Ctx:
    def __init__(self, nc):
        self.nc = nc
        self.eng = {"pe": nc.tensor, "act": nc.scalar, "dve": nc.vector, "pool": nc.gpsimd, "sp": nc.sync}
        self.sem = {k: nc.alloc_semaphore("s_" + k) for k in self.eng}
        self.cnt = {k: 0 for k in self.eng}
        self.waited = {k: {} for k in self.eng}
        self.nsem = 0

    def _wait(self, e, ev, raw=True):
        if ev is None:
            return
        kind, key, val, sem = ev
        if kind == "eng" and key == e and (e == "pe" or not raw):
            return
        w = self.waited[e]
        if w.get(key, 0) >= val:
            return
        w[key] = val
        self.eng[e].wait_ge(sem, val)

    @staticmethod
    def _flat(bufs):
        out = []
        for b in bufs:
            if isinstance(b, (list, tuple)):
                out.extend(Ctx._flat(b))
            else:
                out.append(b)
        return out

    def _deps(self, e, reads, writes):
        reads, writes = self._flat(reads), self._flat(writes)
        for b in reads:
            self._wait(e, b.writer)
        for b in writes:
            self._wait(e, b.writer, raw=False)
            for ev in b.readers.values():
                self._wait(e, ev, raw=False)

    def _record(self, ev, rkey, reads, writes):
        reads, writes = self._flat(reads), self._flat(writes)
        for b in reads:
            b.readers[rkey] = ev
        for b in writes:
            b.writer = ev
            b.readers = {}

    def op(self, e, fn, reads=(), writes=()):
        self._deps(e, reads, writes)
        ins = fn()
        self.cnt[e] += 1
        ins.then_inc(self.sem[e], 1)
        ev = ("eng", e, self.cnt[e], self.sem[e])
        self._record(ev, e, reads, writes)

    def dma(self, q, pairs, sb, reads=(), writes=(), issue=None):
        self._deps(q, reads, writes)
        kind = "sw" if q == "pool" else "hw"
        st = sb.dsems.setdefault(kind, [None, 0])
        if st[0] is None:
            self.nsem += 1
            st[0] = self.nc.alloc_semaphore("d%d" % self.nsem)
        if issue is not None:
            ins = issue()
            st[1] += 16
            ins.then_inc(st[0], 16)
        else:
            for (o, i) in pairs:
                ins = self.eng[q].dma_start(out=o, in_=i)
                st[1] += 16
                ins.then_inc(st[0], 16)
        key = "d%s_%s" % (kind, sb.name)
        ev = ("dma", key, st[1], st[0])
        self._record(ev, key, reads, writes)
        return ev

    def finish(self, bufs):
        for b in bufs:
            self._wait("sp", b.writer)
            for ev in b.readers.values():
                self._wait("sp", ev)


def build_program(dbg=None):
    dbg = dbg or {}
    nc = bass.Bass("TRN2", target_bir_lowering=False)
    cx = Ctx(nc)
    pe, act, dve, pool = nc.tensor, nc.scalar, nc.vector, nc.gpsimd

    def din(name, shape):
        return nc.dram_tensor(name, list(shape), F32, kind="ExternalInput").ap()

    x_in = din("x", [S, D])
    mem_in = din("mem", [NMEM, D])
    mix_norm_g = din("mix_norm_g", [DEPTH, D])
    w_in = din("w_in", [DEPTH, D, NIN])
    b_forget = din("b_forget", [DEPTH, 8])
    fox_q_g = din("fox_q_g", [DEPTH, 64])
    fox_k_g = din("fox_k_g", [DEPTH, 64])
    pool_w = din("pool_w", [DEPTH, 4, 128, 128])
    pool_scale = din("pool_scale", [DEPTH, 512])
    mem_norm_g = din("mem_norm_g", [DEPTH, D])
    w_mem_kv = din("w_mem_kv", [DEPTH, D, 1024])
    mem_q_g = din("mem_q_g", [DEPTH, 128])
    mem_k_g = din("mem_k_g", [DEPTH, 128])
    w_pool_br = din("w_pool_br", [DEPTH, 512, D])
    w_fox_br = din("w_fox_br", [DEPTH, 512, D])
    w_mem_br = din("w_mem_br", [DEPTH, 512, D])
    w_out = din("w_out", [DEPTH, D, D])
    ffn_norm_g = din("ffn_norm_g", [DEPTH, D])
    w_ffn_gu = din("w_ffn_gu", [1, D, 2 * DFF])
    w_ffn_down = din("w_ffn_down", [1, DFF, D])
    w_router = din("w_router", [1, D, NEXP])
    b_router = din("b_router", [1, NEXP])
    w_exp_gu = din("w_exp_gu", [1, NEXP, D, 2 * DEXP])
    w_exp_down = din("w_exp_down", [1, NEXP, DEXP, D])
    out = nc.dram_tensor("out", [S, D], F32, kind="ExternalOutput").ap()
    skind = "ExternalOutput" if dbg else "Internal"
    scrA = nc.dram_tensor("scrA", [S, D], F32, kind=skind).ap()
    scrB = nc.dram_tensor("scrB", [S, D], F32, kind=skind).ap()
    Hg = nc.dram_tensor("Hg", [NSL, D], BF16, kind="Internal").ap()
    Yg = nc.dram_tensor("Yg", [NSL, D], F32, kind="Internal").ap()

    def sb(name, shape, dt):
        return nc.alloc_sbuf_tensor(name, list(shape), dt).ap()

    CACHE_COLS = 8 * S + 32 * 8 * 65
    cache = sb("cache", [128, CACHE_COLS], BF16)
    Kt = [cache[:, h * S:(h + 1) * S] for h in range(8)]
    Vc = cache[:, 8 * S:8 * S + 32 * 520].rearrange("p (b h e) -> p b h e", b=32, h=8)
    aT = cache[:, 0:28 * FT].rearrange("p (f t) -> p f t", f=28)
    yT = cache[:, 28 * FT:28 * FT + 2 * 8 * FT].bitcast(F32).rearrange("p (c t) -> p c t", c=8)
    b_cache = Buf("cache_all")
    aTr = cache[:, 0:28 * CAP].rearrange("p (f t) -> p f t", f=28)
    yTs = cache[:, 28 * CAP:28 * CAP + 2 * 4 * CAP].bitcast(F32).rearrange("p (c t) -> p c t", c=4)

    ring = [sb("ring%d" % i, [128, SLOT_COLS], BF16) for i in range(NSLOT)]
    b_ring = [Buf("ring%d" % i) for i in range(NSLOT)]
    ring_i = [0]

    hT = sb("hT", [128, 8, TT], BF16)
    b_hT = Buf("hT")
    Qt = sb("Qt", [128, 8, TT], BF16)
    b_QtAll = Buf("Qt")
    b_Qt = [b_QtAll for h in range(8)]
    hTh = [hT, Qt]
    b_hTh = [b_hT, b_QtAll]
    b_Kt = [[Buf("Kt%d_%d" % (h, i)) for i in range(NT)] for h in range(8)]
    b_Vc = [Buf("Vc%d" % i) for i in range(NT)]
    xs = [sb("xs%d" % i, [128, D], F32) for i in range(2)]
    b_xs = [Buf("xs%d" % i) for i in range(2)]
    hbs = [sb("hb%d" % i, [128, D], BF16) for i in range(2)]
    b_hbs = [Buf("hb%d" % i) for i in range(2)]
    outs = xs
    b_outs = b_xs
    gcols = sb("gcols", [128, 3, 8], F32)
    b_gcols = Buf("gcols")
    brT = sb("brT", [128, 3, 4, TT], BF16)
    b_brT = [Buf("brT%d" % i) for i in range(3)]
    mergedT = sb("mergedT", [128, 8, TT], BF16)
    b_merged = Buf("merged")
    efs = [mergedT[:, k, :] for k in range(6)]
    b_efs = [Buf("efs%d" % k) for k in range(6)]
    efs_i = [0]

    def ef_next():
        k = efs_i[0] % 6
        efs_i[0] += 1
        return efs[k], b_efs[k]
    macc = sb("macc", [128, 2, TT], F32)
    b_maccAll = Buf("macc")
    b_macc = [b_maccAll for i in range(4)]
    NPT = 4
    pT = [sb("pT%d" % i, [128, TT], BF16) for i in range(NPT)]
    b_pT = [Buf("pT%d" % i) for i in range(NPT)]
    pT_i = [0]

    def pt_next():
        k = pT_i[0] % NPT
        pT_i[0] += 1
        return k
    rec = sb("rec", [128, TT], F32)
    b_rec = Buf("rec")
    rs, b_rs = rec, b_rec
    gt = [sb("gt%d" % i, [128, TT], F32) for i in range(2)]
    b_gt = [Buf("gt%d" % i) for i in range(2)]
    gt_i = [0]
    tmpf = sb("tmpf", [128, 16], F32)
    b_tmpf = Buf("tmpf")
    U = sb("U", [128, 16 + TT], F32)
    b_U = Buf("U")
    La = sb("La", [128, 16 + TT], F32)
    b_La = Buf("La")
    Lb = sb("Lb", [128, 16 + TT], F32)
    b_Lb = Buf("Lb")
    halo = sb("halo", [128, 4, 16], F32)
    b_halo = Buf("halo")
    tf = sb("tf", [128, 32], F32)
    b_tf = Buf("tf")
    nl = sb("nl", [128, 32], F32)
    b_nl = Buf("nl")
    Cs = sb("Cs", [8, TT], F32)
    b_Cs = Buf("Cs")
    carry = sb("carry", [8, 1], F32)
    b_carry = Buf("carry")
    r1 = sb("r1", [8, TT], F32)
    b_r1 = Buf("r1")
    lot = sb("lot", [8, TT], BF16)
    b_lot = Buf("lot")
    PIE = sb("PIE", [128, TT], BF16)
    b_PIE = Buf("PIE")
    memT = macc[:, 0:2, :].rearrange("p a t -> p (a t)").bitcast(BF16).rearrange("p (k m) -> p k m", k=8)
    b_memT = b_maccAll
    KmT = sb("KmT", [128, 4, NMEM], BF16)
    b_KmT = Buf("KmT")
    Vm = sb("Vm", [128, 2, 512], BF16)
    b_Vm = Buf("Vm")
    poolw = sb("poolw", [128, 4, 128], BF16)
    b_poolw = Buf("poolw")
    cols = sb("cols", [128, 16], F32)
    b_cols = Buf("cols")
    bfB = sb("bfB", [128, 8], F32)
    b_bfB = Buf("bfB")
    brB = sb("brB", [128, 8], F32)
    b_brB = Buf("brB")
    cwB = [brT[:, i, :, :].rearrange("p a t -> p (a t)").bitcast(F32) for i in range(2)]
    b_cwB = [b_brT[i] for i in range(2)]
    cwT = mergedT[0:8, 0:4, :].rearrange("p a t -> p (a t)").bitcast(F32)
    cwTm = mergedT[0:8, 4:8, :].rearrange("p a t -> p (a t)").bitcast(F32)
    b_cwT = b_merged
    rt = sb("rt", [128, 96], F32)
    b_rt = Buf("rt")
    gidx = sb("gidx", [128, 32, 2], I32)
    b_gidx = Buf("gidx")
    sidx = [sb("sidx%d" % i, [128, 2], I32) for i in range(2)]
    b_sidx = [Buf("sidx%d" % i) for i in range(2)]
    wts = sb("wts", [128, 32, 2], F32)
    b_wts = Buf("wts")
    carry_bc = sb("carry_bc", [128, 8], F32)
    b_cbc = Buf("carry_bc")
    eCm1 = sb("eCm1", [128, 8], F32)
    wf = sb("wf", [128, 8, 8], BF16)
    b_wf = Buf("wf")
    wr = sb("wr", [128, 8, 8], BF16)
    b_wr = Buf("wr")
    ss1 = sb("ss1", [128, 2], F32)
    b_ss1 = Buf("ss1")
    ident_b = sb("ident_b", [128, 128], BF16)
    ident_f = sb("ident_f", [128, 128], F32)
    blk64 = sb("blk64", [128, 128], BF16)
    ones_b = sb("ones_b", [128, 128], BF16)
    ones_f = sb("ones_f", [128, 128], F32)
    triU = sb("triU", [128, 128], F32)
    maskT = sb("maskT", [128, 128], BF16)
    SelK = sb("SelK", [128, 8, 70], BF16)
    SelQ = sb("SelQ", [128, 8, 70], BF16)
    invc = sb("invc", [128, 4, 16], F32)
    epsc = sb("epsc", [128, 1], F32)
    b_const = Buf("const")

    pbank = [nc.alloc_psum_tensor("pb%d" % i, [128, 512], F32).ap() for i in range(8)]
    b_pb = [Buf("pb%d" % i) for i in range(8)]
    pp = {"A": [0, 1, 2], "S": [3, 4], "O": [5, 6], "X": [7], "SA": [3, 4, 0], "EF": [1, 2]}
    pp_i = {k: 0 for k in pp}

    def pget(kind):
        lst = pp[kind]
        i = lst[pp_i[kind] % len(lst)]
        pp_i[kind] += 1
        return pbank[i], b_pb[i]

    MARKS = []

    def MARK(name):
        MARKS.append((name, dict(cx.cnt)))

    def setup_consts():
        def P(fn, bufs=None):
            cx.op("pool", fn, reads=bufs or [b_const], writes=bufs or [b_const])

        def eye(t):
            P(lambda: pool.memset(t[:], 1.0))
            P(lambda: pool.affine_select(out=t[:], in_=t[:], pattern=[[-1, 128]], compare_op=ALU.is_equal,
                                         fill=0.0, base=0, channel_multiplier=1))
        eye(ident_b)
        eye(ident_f)
        P(lambda: pool.memset(ones_b[:], 1.0))
        P(lambda: pool.memset(ones_f[:], 1.0))
        P(lambda: pool.memset(blk64[:], 0.0))
        P(lambda: pool.memset(blk64[0:64, 0:64], 1.0))
        P(lambda: pool.memset(blk64[64:128, 64:128], 1.0))
        P(lambda: pool.memset(triU[:], 1.0))
        P(lambda: pool.affine_select(out=triU[:], in_=triU[:], pattern=[[1, 128]], compare_op=ALU.is_ge,
                                     fill=0.0, base=0, channel_multiplier=-1))
        P(lambda: pool.memset(maskT[:], 0.0))
        P(lambda: pool.affine_select(out=maskT[:], in_=maskT[:], pattern=[[1, 128]], compare_op=ALU.is_ge,
                                     fill=-30000.0, base=0, channel_multiplier=-1))
        P(lambda: pool.memset(SelK[:], 1.0))
        P(lambda: pool.memset(SelK[:, :, 0:64], 0.0))
        for j in range(3):
            P(lambda j=j: pool.affine_select(out=SelK[:, :, 64 + j:65 + j], in_=SelK[:, :, 64 + j:65 + j],
                                             pattern=[[-1, 8], [0, 1]], compare_op=ALU.is_equal, fill=0.0,
                                             base=-32 * j, channel_multiplier=1))
        P(lambda: pool.affine_select(out=SelK[:, :, 67:70], in_=SelK[:, :, 67:70], pattern=[[0, 8], [0, 3]],
                                     compare_op=ALU.is_equal, fill=0.0, base=-96, channel_multiplier=1))
        P(lambda: pool.memset(SelQ[:], 1.0))
        P(lambda: pool.memset(SelQ[:, :, 0:64], 0.0))
        P(lambda: pool.memset(SelQ[:, :, 67:70], -1.0))
        P(lambda: pool.affine_select(out=SelQ[:, :, 64:67], in_=SelQ[:, :, 64:67], pattern=[[0, 8], [0, 3]],
                                     compare_op=ALU.is_equal, fill=0.0, base=-96, channel_multiplier=1))
        for j in range(3):
            P(lambda j=j: pool.affine_select(out=SelQ[:, :, 67 + j:68 + j], in_=SelQ[:, :, 67 + j:68 + j],
                                             pattern=[[-1, 8], [0, 1]], compare_op=ALU.is_equal, fill=0.0,
                                             base=-32 * j, channel_multiplier=1))
        for g in range(4):
            w = 2 ** (g + 1)
            P(lambda g=g: pool.memset(invc[:, g, :], 1.0))
            for t in range(min(w - 1, 16)):
                P(lambda g=g, t=t, w=w: pool.memset(invc[:, g, t:t + 1], float(w) / (t + 1)))
        P(lambda: pool.memset(epsc[:], EPS))
        for e_ in range(NEXP):
            P(lambda e_=e_: pool.memset(eCm1[:, e_:e_ + 1], float(e_ * CAP - 1)))
        P(lambda: pool.memset(hbs[0][:], 0.0), [b_hbs[0]])
        P(lambda: pool.memset(PIE[:], 0.0), [b_PIE])
        P(lambda: pool.memset(PIE[96:97, :], 1.0), [b_PIE])
        P(lambda: pool.memset(halo[:], 0.0), [b_halo])
        P(lambda: pool.memset(carry[:], 0.0), [b_carry])
        P(lambda: pool.memset(Vc[:, :, :, 64:65], 1.0), b_Vc)

    setup_consts()
    bcreg = nc.gpsimd.alloc_register("bcreg")
    nc.gpsimd.reg_mov(bcreg, NSL - 1)

    b_half = [Buf("ringh%d" % i) for i in range(2 * NSLOT)]
    HALF = SLOT_COLS // 2

    def ring_load(src_ap, shape3, cv=None):
        a, b = shape3
        n_el = a * b
        if n_el <= HALF:
            h = ring_i[0] % (2 * NSLOT)
            ring_i[0] += 1
            view = ring[h // 2][:, (h % 2) * HALF:(h % 2) * HALF + n_el].rearrange("p (a b) -> p a b", a=a)
            bufs = [b_half[h]]
        else:
            if ring_i[0] % 2:
                ring_i[0] += 1
            h = ring_i[0] % (2 * NSLOT)
            ring_i[0] += 2
            view = ring[h // 2][:, 0:n_el].rearrange("p (a b) -> p a b", a=a)
            bufs = [b_half[h], b_half[h + 1]]
        if cv is None:
            cx.dma("pool", [(view, src_ap)], bufs[0], writes=bufs)
        else:
            cx.dma("sp", [(view, src_ap)], bufs[0], reads=[cv], writes=bufs)
        return view, bufs

    CV = {}

    def convert(name, src2d):
        R_, C_ = src2d.shape
        dstc = nc.dram_tensor("bf_" + name, [R_, C_], BF16, kind="Internal").ap()
        bcv = Buf("cv_" + name)
        cx.dma("pool", [(dstc[r:r + 128, :], src2d[r:r + 128, :]) for r in range(0, R_, 128)], bcv, writes=[bcv])
        CV[name] = (dstc, bcv)

    def load_layer_consts(l):
        gprs = []
        for gi2, gsrc in enumerate((mix_norm_g, mem_norm_g, ffn_norm_g)):
            for k in range(8):
                gprs.append((gcols[:, gi2, k:k + 1], gsrc[l, k * 128:(k + 1) * 128].rearrange("(p o) -> p o", o=1)))
        cx.dma("sp", gprs, b_gcols, writes=[b_gcols])
        cx.dma("sp", [(bfB[:], b_forget[l:l + 1, :].to_broadcast([128, 8]))], b_bfB, writes=[b_bfB])
        prs = []
        qg = fox_q_g[l].rearrange("(p o) -> p o", o=1)
        kg = fox_k_g[l].rearrange("(p o) -> p o", o=1)
        for j in range(2):
            prs.append((cols[j * 64:(j + 1) * 64, 0:1], qg))
            prs.append((cols[j * 64:(j + 1) * 64, 1:2], kg))
        prs.append((cols[:, 2:3], mem_q_g[l].rearrange("(p o) -> p o", o=1)))
        prs.append((cols[:, 3:4], mem_k_g[l].rearrange("(p o) -> p o", o=1)))
        for g in range(4):
            prs.append((cols[:, 4 + g:5 + g], pool_scale[l, g * 128:(g + 1) * 128].rearrange("(p o) -> p o", o=1)))
        cx.dma("sp", prs, b_cols, writes=[b_cols])
        cx.op("dve", lambda: dve.tensor_scalar(out=cols[:, 0:1], in0=cols[:, 0:1], scalar1=0.125, scalar2=None,
                                               op0=ALU.mult), reads=[b_cols], writes=[b_cols])
        cx.op("dve", lambda: dve.tensor_scalar(out=cols[:, 2:3], in0=cols[:, 2:3], scalar1=float(128 ** -0.5),
                                               scalar2=None, op0=ALU.mult), reads=[b_cols], writes=[b_cols])
        cx.dma("pool", [(poolw[:], pool_w[l].rearrange("g p n -> p g n"))], b_poolw, writes=[b_poolw])

    xs_i = [0]

    def norm_transpose(src_rows, gsel, dstT, b_dst):
        i = xs_i[0] % 2
        xs_i[0] += 1
        xt, bx = xs[i], b_xs[i]
        hb, b_hb = hbs[i], b_hbs[i]
        cx.dma("sp", [(xt[:], src_rows)], bx, writes=[bx])
        cx.op("act", lambda: act.activation(out=hb[:], in_=xt[:], func=AF.Square, accum_out=ss1[:, 0:1]),
              reads=[bx], writes=[b_hb, b_ss1])
        cx.op("act", lambda: act.activation(out=ss1[:, 1:2], in_=ss1[:, 0:1], func=AF.Sqrt, bias=epsc[:, 0:1],
                                            scale=1.0 / D), reads=[b_ss1, b_const], writes=[b_ss1])
        cx.op("dve", lambda: dve.reciprocal(out=ss1[:, 1:2], in_=ss1[:, 1:2]), reads=[b_ss1], writes=[b_ss1])
        cx.op("act", lambda: act.activation(out=hb[:], in_=xt[:], func=AF.Identity, scale=ss1[:, 1:2]),
              reads=[bx, b_ss1], writes=[b_hb])
        pt, bp = pget("X")
        ptb = pt.bitcast(BF16)

        def f():
            for k in range(8):
                ins = pe.transpose(ptb[:, k * 128:(k + 1) * 128], hb[:, k * 128:(k + 1) * 128], ident_b[:])
            return ins
        cx.op("pe", f, reads=[b_hb, b_const], writes=[bp])
        cx.op("dve", lambda: dve.tensor_tensor(out=dstT, in0=ptb[:, 0:1024].rearrange("p (k t) -> p k t", k=8),
                                               in1=gcols[:, gsel, :].unsqueeze(2).to_broadcast([128, 8, 128]), op=ALU.mult),
              reads=[bp, b_gcols], writes=[b_dst])

    def pipelined(items, produce, consume, la=1):
        outs_ = {}
        n_ = len(items)
        for t_ in range(n_ + la):
            if t_ < n_:
                outs_[t_] = produce(items[t_])
            if t_ - la >= 0:
                consume(items[t_ - la], outs_.pop(t_ - la))

    def mm_group(out_ap, b_out, pairs, reads, first=True, last=True):
        def f():
            n = len(pairs)
            for idx, (l, r) in enumerate(pairs):
                ins = pe.matmul(out_ap, lhsT=l, rhs=r, start=(first and idx == 0), stop=(last and idx == n - 1))
            return ins
        cx.op("pe", f, reads=reads, writes=[b_out])

    def head_norm(ps, bps, ones_mat, inv_n, gcol_idx, dsts, b_dsts, n=TT):
        kq = pt_next()
        sqs, b_sqs = pT[kq], b_pT[kq]
        cx.op("act", lambda: act.activation(out=sqs[:, 0:n], in_=ps[:, 0:n], func=AF.Square), reads=[bps], writes=[b_sqs])
        p2, bp2 = pget("X")
        mm_group(p2[:, 0:n], bp2, [(ones_mat[:], sqs[:, 0:n])], [b_sqs, b_const])
        cx.op("act", lambda: act.activation(out=rs[:, 0:n], in_=p2[:, 0:n], func=AF.Sqrt, bias=epsc[:, 0:1], scale=inv_n),
              reads=[bp2, b_const], writes=[b_rs])
        cx.op("dve", lambda: dve.reciprocal(out=rs[:, 0:n], in_=rs[:, 0:n]), reads=[b_rs], writes=[b_rs])
        for (lo, hi, o), bd in zip(dsts, b_dsts):
            cx.op("dve", lambda lo=lo, hi=hi, o=o: dve.scalar_tensor_tensor(
                out=o, in0=ps[lo:hi, 0:n], scalar=cols[lo:hi, gcol_idx:gcol_idx + 1], in1=rs[lo:hi, 0:n],
                op0=ALU.mult, op1=ALU.mult), reads=[bps, b_rs, b_cols], writes=[bd])

    def mem_kv(l):
        for blk in range(2):
            norm_transpose(mem_in[blk * 128:(blk + 1) * 128, :], 1,
                           memT[:, :, blk * 128:(blk + 1) * 128], b_memT)
        wkv_src, wkv_cv = CV["w_mem_kv%d" % l]
        wk, bwk = ring_load(wkv_src.rearrange("(k p) n -> p k n", p=128)[:, :, 0:512], (8, 512), wkv_cv)
        for hm in range(4):
            ps, bps = pget("A")
            mm_group(ps[:, 0:NMEM], bps, [(wk[:, k, hm * 128:(hm + 1) * 128], memT[:, k, :]) for k in range(8)],
                     [bwk, b_memT])
            head_norm(ps, bps, ones_b, 1.0 / 128, 3, [(0, 128, KmT[:, hm, :])], [b_KmT], n=NMEM)
        wv, bwv = ring_load(wkv_src.rearrange("(k p) n -> p k n", p=128)[:, :, 512:1024], (8, 512), wkv_cv)
        for blk in range(2):
            ps, bps = pget("A")
            mm_group(ps[:], bps, [(memT[:, k, blk * 128:(blk + 1) * 128], wv[:, k, :]) for k in range(8)],
                     [bwv, b_memT])
            cx.op("act", lambda blk=blk, ps=ps: act.copy(out=Vm[:, blk, :], in_=ps[:]), reads=[bps], writes=[b_Vm])

    def mixer_tile(l, i, src, dst):
        t0 = i * TT
        MARK('L%d T%d A' % (l, i))
        win = CV["w_in%d" % l][0].rearrange("(k p) n -> p k n", p=128)
        cvin = CV["w_in%d" % l][1]
        for blk in range(4):
            norm_transpose(src[t0 + blk * 128:t0 + (blk + 1) * 128, :], 0,
                           hT[:, :, blk * 128:(blk + 1) * 128], b_hT)
        MARK('L%d T%d B' % (l, i))
        itemsB = [(off, gi, is_q, c) for (off, gi, is_q) in ((OFF_K, 1, False), (OFF_Q, 0, True)) for c in range(4)]
        wcur = {}

        def prodB(it):
            off, gi, is_q, c = it
            if c == 0:
                wcur["w"] = ring_load(win[:, :, off:off + 512], (8, 512), cvin)
            w, bw = wcur["w"]
            ps, bps = pget("A")
            mm_group(ps[:], bps, [(w[:, k, c * 128:(c + 1) * 128], hT[:, k, 0:TT]) for k in range(8)], [bw, b_hT])
            return ps, bps

        def consB(it, o):
            off, gi, is_q, c = it
            ps, bps = o
            if is_q:
                dsts = [(j * 64, (j + 1) * 64, Qt[0:64, 2 * c + j, :]) for j in range(2)]
                bds = [b_Qt[2 * c + j] for j in range(2)]
            else:
                dsts = [(j * 64, (j + 1) * 64, Kt[2 * c + j][0:64, t0:t0 + TT]) for j in range(2)]
                bds = [b_Kt[2 * c + j][i] for j in range(2)]
            head_norm(ps, bps, blk64, 1.0 / 64, gi, dsts, bds)
        pipelined(itemsB, prodB, consB)
        MARK('L%d T%d C' % (l, i))
        w, bw = ring_load(win[:, :, OFF_V:OFF_V + 512], (8, 512), cvin)
        cx.dma("sp", [(wf[:], win[:, :, OFF_F:OFF_F + 8])], b_wf, reads=[cvin], writes=[b_wf])
        for blk in range(4):
            ps, bps = pget("A")
            mm_group(ps[:], bps, [(hT[:, k, blk * 128:(blk + 1) * 128], w[:, k, 0:512]) for k in range(8)], [bw, b_hT])
            cx.op("act", lambda blk=blk, ps=ps: act.copy(out=Vc[:, i * 4 + blk, :, 0:64],
                                                          in_=ps[:].rearrange("p (h e) -> p h e", h=8)),
                  reads=[bps], writes=[b_Vc[i]])
        pf, bpf = pget("X")
        for blk in range(4):
            mm_group(pf[:, blk * 8:(blk + 1) * 8], bpf,
                     [(hT[:, k, blk * 128:(blk + 1) * 128], wf[:, k, :]) for k in range(8)], [b_wf, b_hT])
        cx.op("dve", lambda: dve.tensor_tensor(out=tf[:].rearrange("p (b e) -> p b e", b=4),
                                               in0=pf[:, 0:32].rearrange("p (b e) -> p b e", b=4),
                                               in1=bfB[:].unsqueeze(1).to_broadcast([128, 4, 8]), op=ALU.add),
              reads=[bpf, b_bfB], writes=[b_tf])
        cx.op("act", lambda: act.activation(out=tf[:], in_=tf[:], func=AF.Exp, scale=-1.0), reads=[b_tf], writes=[b_tf])
        cx.op("act", lambda: act.activation(out=nl[:], in_=tf[:], func=AF.Ln, bias=1.0, scale=1.0),
              reads=[b_tf], writes=[b_nl])
        pc, bpc = pget("X")
        for blk in range(4):
            prs_ = [(nl[:, b2 * 8:(b2 + 1) * 8], ones_f[:]) for b2 in range(blk)] + [(nl[:, blk * 8:(blk + 1) * 8], triU[:])]
            mm_group(pc[0:8, blk * 128:(blk + 1) * 128], bpc, prs_, [b_nl, b_const])
        cx.op("dve", lambda: dve.tensor_scalar(out=Cs[0:8, :], in0=pc[0:8, :], scalar1=carry[0:8, 0:1],
                                               scalar2=None, op0=ALU.add), reads=[bpc, b_carry], writes=[b_Cs])
        cx.op("act", lambda: act.copy(out=carry[0:8, 0:1], in_=Cs[0:8, TT - 1:TT]), reads=[b_Cs], writes=[b_carry])
        cx.op("dve", lambda: dve.tensor_copy(out=PIE[0:8, :], in_=Cs[0:8, :]), reads=[b_Cs], writes=[b_PIE])
        cx.op("dve", lambda: dve.tensor_tensor(out=r1[0:8, :], in0=Cs[0:8, :], in1=PIE[0:8, :], op=ALU.subtract),
              reads=[b_Cs, b_PIE], writes=[b_r1])
        cx.op("dve", lambda: dve.tensor_copy(out=lot[0:8, :], in_=r1[0:8, :]), reads=[b_r1], writes=[b_lot])
        cx.op("dve", lambda: dve.tensor_copy(out=PIE[32:40, :], in_=lot[0:8, :]), reads=[b_lot], writes=[b_PIE])
        cx.op("dve", lambda: dve.tensor_tensor(out=r1[0:8, :], in0=r1[0:8, :], in1=lot[0:8, :], op=ALU.subtract),
              reads=[b_r1, b_lot], writes=[b_r1])
        cx.op("dve", lambda: dve.tensor_copy(out=PIE[64:72, :], in_=r1[0:8, :]), reads=[b_r1], writes=[b_PIE])
        for h in range(8):
            for (Sel, dst_ap, bd) in ((SelK, Kt[h][64:70, t0:t0 + TT], b_Kt[h][i]), (SelQ, Qt[64:70, h, :], b_Qt[h])):
                pa, bpa = pget("S") if (h % 2 == 0) else pget("O")
                mm_group(pa[0:70, :], bpa, [(Sel[:, h, :], PIE[:])], [b_PIE, b_const])
                cx.op("act", lambda pa=pa, dst_ap=dst_ap: act.copy(out=dst_ap, in_=pa[64:70, :]), reads=[bpa], writes=[bd])
        MARK('L%d T%d D' % (l, i))

        def head_norm_g(ps, bps, ones_mat, inv_n, gcol_idx, out_ap, bd, n=TT):
            sq_, bsq = ef_next()
            cx.op("act", lambda: act.activation(out=sq_[:, 0:n], in_=ps[:, 0:n], func=AF.Square), reads=[bps], writes=[bsq])
            yield
            p2, bp2 = pget("EF")
            mm_group(p2[:, 0:n], bp2, [(ones_mat[:], sq_[:, 0:n])], [bsq, b_const])
            yield
            cx.op("act", lambda: act.activation(out=rs[:, 0:n], in_=p2[:, 0:n], func=AF.Sqrt, bias=epsc[:, 0:1], scale=inv_n),
                  reads=[bp2, b_const], writes=[b_rs])
            yield
            cx.op("dve", lambda: dve.reciprocal(out=rs[:, 0:n], in_=rs[:, 0:n]), reads=[b_rs], writes=[b_rs])
            yield
            cx.op("dve", lambda: dve.scalar_tensor_tensor(out=out_ap, in0=ps[:, 0:n], scalar=cols[:, gcol_idx:gcol_idx + 1],
                                                          in1=rs[:, 0:n], op0=ALU.mult, op1=ALU.mult),
                  reads=[bps, b_rs, b_cols], writes=[bd])
            yield

        def ef_steps():
            w, bw = ring_load(win[:, :, OFF_POOL:OFF_POOL + 512], (8, 512), cvin)
            yield
            for g in range(4):
                ps, bps = pget("EF")
                mm_group(ps[:], bps, [(w[:, k, g * 128:(g + 1) * 128], hT[:, k, 0:TT]) for k in range(8)], [bw, b_hT])
                yield
                cx.op("act", lambda ps=ps: act.copy(out=U[:, 16:16 + TT], in_=ps[:]), reads=[bps], writes=[b_U])
                cx.op("dve", lambda g=g: dve.tensor_copy(out=U[:, 0:16], in_=halo[:, g, :]), reads=[b_halo], writes=[b_U])
                yield
                cx.op("dve", lambda g=g: dve.tensor_copy(out=halo[:, g, :], in_=U[:, TT:TT + 16]), reads=[b_U], writes=[b_halo])
                E = 16 + TT
                srcL, bsrc = U, b_U
                lo = 0
                bufs = [(La, b_La), (Lb, b_Lb)]
                for lev in range(g + 1):
                    sh = 2 ** lev
                    dL, bdL = bufs[lev % 2]
                    lo2 = lo + sh
                    cx.op("dve", lambda dL=dL, srcL=srcL, lo2=lo2, sh=sh: dve.tensor_tensor(
                        out=dL[:, lo2:E], in0=srcL[:, lo2:E], in1=srcL[:, lo2 - sh:E - sh], op=ALU.add),
                        reads=[bsrc], writes=[bdL])
                    srcL, bsrc, lo = dL, bdL, lo2
                    yield
                wv_ = 2 ** (g + 1)
                dT, b_dT = ef_next()
                if i == 0:
                    cx.op("dve", lambda srcL=srcL, g=g: dve.tensor_tensor(out=srcL[:, 16:32], in0=srcL[:, 16:32],
                                                                        in1=invc[:, g, :], op=ALU.mult),
                          reads=[bsrc, b_const], writes=[bsrc])
                    yield
                cx.op("dve", lambda srcL=srcL, wv_=wv_, dT=dT: dve.scalar_tensor_tensor(
                    out=dT[:], in0=srcL[:, 16:E], scalar=1.0 / wv_, in1=U[:, 16:E], op0=ALU.mult, op1=ALU.subtract),
                    reads=[bsrc, b_U], writes=[b_dT])
                yield
                p2, bp2 = pget("EF")
                mm_group(p2[:], bp2, [(poolw[:, g, :], dT[:])], [b_poolw, b_dT])
                yield
                cx.op("act", lambda p2=p2, g=g: act.activation(out=brT[:, 0, g, :], in_=p2[:], func=AF.Identity,
                                                              scale=cols[:, 4 + g:5 + g]),
                      reads=[bp2, b_cols], writes=[b_brT[0]])
                yield
            w2, bw2 = ring_load(win[:, :, OFF_MQ:OFF_MQ + 512], (8, 512), cvin)
            yield
            for hm in range(4):
                ps, bps = pget("EF")
                mm_group(ps[:], bps, [(w2[:, k, hm * 128:(hm + 1) * 128], hT[:, k, 0:TT]) for k in range(8)], [bw2, b_hT])
                yield
                qm, b_qm = ef_next()
                yield from head_norm_g(ps, bps, ones_b, 1.0 / 128, 2, qm[:], b_qm)
                exs = []
                for mc in range(2):
                    s_, bs_ = pget("EF")
                    mm_group(s_[:], bs_, [(KmT[:, hm, mc * 128:(mc + 1) * 128], qm[:])], [b_KmT, b_qm])
                    yield
                    ex, bex = ef_next()
                    cx.op("act", lambda s_=s_, ex=ex: act.activation(out=ex[:], in_=s_[:], func=AF.Exp),
                          reads=[bs_], writes=[bex])
                    exs.append((ex, bex))
                    yield
                po, bpo = pget("EF")
                mm_group(po[:], bpo, [(Vm[:, mc, hm * 128:(hm + 1) * 128], exs[mc][0][:]) for mc in range(2)],
                         [b_Vm, exs[0][1], exs[1][1]])
                yield
                pq, bpq = pget("EF")
                mm_group(pq[:], bpq, [(ones_b[:], exs[mc][0][:]) for mc in range(2)], [b_const, exs[0][1], exs[1][1]])
                yield
                cx.op("dve", lambda pq=pq: dve.reciprocal(out=rec[:], in_=pq[:]), reads=[bpq], writes=[b_rec])
                yield
                cx.op("dve", lambda po=po, hm=hm: dve.tensor_tensor(out=brT[:, 2, hm, :], in0=po[:], in1=rec[:], op=ALU.mult),
                      reads=[bpo, b_rec], writes=[b_brT[2]])
                yield

        nkb = 4 * (i + 1)
        blocks = [(h, j) for h in range(8) for j in range(nkb)]
        NB_ = len(blocks)
        hstate = {}
        pend = {}
        fin_q = []

        def emit_S(n):
            h, j = blocks[n]
            jj = j - 4 * i
            q0 = max(0, jj) * 128
            ps, bps = pget("SA")

            def fs():
                ins = pe.matmul(ps[:, q0:TT], lhsT=Kt[h][0:70, j * 128:(j + 1) * 128], rhs=Qt[0:70, h, q0:TT],
                                start=True, stop=(jj < 0))
                if jj >= 0:
                    ins = pe.matmul(ps[:, q0:q0 + 128], lhsT=ident_b[:], rhs=maskT[:], start=False, stop=True)
                return ins
            cx.op("pe", fs, reads=[b_Kt[h][j // 4], b_Qt[h], b_const], writes=[bps])
            k3 = pt_next()
            cx.op("act", lambda: act.activation(out=pT[k3][:, q0:TT], in_=ps[:, q0:TT], func=AF.Exp),
                  reads=[bps], writes=[b_pT[k3]])
            pend[n] = (k3, q0)

        def emit_PV(n):
            h, j = blocks[n]
            k3, q0 = pend.pop(n)
            if j == 0:
                gb_ = gt_i[0] % 2
                gt_i[0] += 1
                hstate[h] = pget("O") + (gt[gb_], b_gt[gb_])
            po, bpo, gtt, bgtt = hstate[h]
            cx.op("pe", lambda: pe.matmul(po[0:65, q0:TT], lhsT=Vc[:, j, h, :], rhs=pT[k3][:, q0:TT],
                                          start=(j == 0), stop=(j == nkb - 1)),
                  reads=[b_Vc[j // 4], b_pT[k3]], writes=[bpo])
            if j == nkb - 1:
                cx.op("dve", lambda: dve.reciprocal(out=gtt[64:65, :], in_=po[64:65, :]), reads=[bpo], writes=[bgtt])
                fin_q.append((n + 4, h))

        def emit_fin(h):
            po, bpo, gtt, bgtt = hstate[h]
            pb_, bpb = pget("X")
            mm_group(pb_[0:64, :], bpb, [(ones_f[64:65, 0:64], gtt[64:65, :])], [bgtt, b_const])
            cx.op("dve", lambda: dve.tensor_copy(out=gtt[0:64, :], in_=pb_[0:64, :]), reads=[bpb], writes=[bgtt])
            hp = (h % 2) * 64
            cx.op("dve", lambda: dve.tensor_tensor(out=brT[hp:hp + 64, 1, h // 2, :], in0=po[0:64, :],
                                                   in1=gtt[0:64, :], op=ALU.mult),
                  reads=[bpo, bgtt], writes=[b_brT[1]])

        for e_ in ("dve", "act"):
            cx._deps(e_, [], [b_merged])
        gen = ef_steps()
        spb = max(1, -(-120 // NB_))
        LA_ = 2
        for n in range(NB_ + LA_):
            if n < NB_:
                emit_S(n)
            if n >= LA_:
                emit_PV(n - LA_)
            while fin_q and fin_q[0][0] <= n:
                emit_fin(fin_q.pop(0)[1])
            for _ in range(spb):
                next(gen, None)
        while fin_q:
            emit_fin(fin_q.pop(0)[1])
        for _ in gen:
            pass
        for e_ in ("dve", "act", "pe", "pool"):
            cx._deps(e_, [], b_efs)
        MARK('L%d T%d G' % (l, i))
        wbrs = (w_pool_br, w_fox_br, w_mem_br)
        for hc in range(4):
            for br in range(3):
                goff = OFF_G + br * 1024 + hc * 256
                wg, bwg = ring_load(win[:, :, goff:goff + 256], (8, 256), cvin)
                wbsrc, wbcv = CV["w_br%d_%d" % (br, l)]
                wb, bwb = ring_load(wbsrc.rearrange("(k p) n -> p k n", p=128)[:, :, hc * 256:(hc + 1) * 256], (4, 256), wbcv)
                for cc in range(2):
                    c = hc * 2 + cc
                    ps, bps = pget("A")
                    mm_group(ps[:], bps, [(wg[:, k, cc * 128:(cc + 1) * 128], hT[:, k, 0:TT]) for k in range(8)], [bwg, b_hT])
                    gi_ = gt_i[0] % 2
                    gt_i[0] += 1
                    cx.op("act", lambda ps=ps, gi_=gi_: act.activation(out=gt[gi_][:], in_=ps[:], func=AF.Sigmoid),
                          reads=[bps], writes=[b_gt[gi_]])
                    p2, bp2 = pget("S")
                    mm_group(p2[:], bp2, [(wb[:, k, cc * 128:(cc + 1) * 128], brT[:, br, k, :]) for k in range(4)],
                             [bwb, b_brT[br]])
                    if br == 0:
                        cx.op("dve", lambda cc=cc, gi_=gi_, p2=p2: dve.tensor_tensor(out=macc[:, cc, :], in0=gt[gi_][:],
                                                                                in1=p2[:], op=ALU.mult),
                              reads=[b_gt[gi_], bp2], writes=[b_macc[cc]])
                    else:
                        cx.op("dve", lambda gi_=gi_, p2=p2: dve.tensor_tensor(out=gt[gi_][:], in0=gt[gi_][:], in1=p2[:],
                                                                          op=ALU.mult),
                              reads=[b_gt[gi_], bp2], writes=[b_gt[gi_]])
                        if br == 1:
                            cx.op("pool", lambda cc=cc, gi_=gi_: pool.tensor_tensor(out=macc[:, cc, :], in0=macc[:, cc, :],
                                                                                in1=gt[gi_][:], op=ALU.add),
                                  reads=[b_gt[gi_], b_macc[cc]], writes=[b_macc[cc]])
                        else:
                            cx.op("pool", lambda cc=cc, gi_=gi_, c=c: pool.tensor_tensor(out=mergedT[:, c, :], in0=macc[:, cc, :],
                                                                                     in1=gt[gi_][:], op=ALU.add),
                                  reads=[b_gt[gi_], b_macc[cc]], writes=[b_merged])
        MARK('L%d T%d H' % (l, i))
        wo_v = CV["w_out%d" % l][0].rearrange("(k p) n -> p k n", p=128)
        wocv = CV["w_out%d" % l][1]
        for half in range(2):
            wo, bwo = ring_load(wo_v[:, :, half * 512:(half + 1) * 512], (8, 512), wocv)
            for blk in range(4):
                r0 = t0 + blk * 128
                xi = xs_i[0] % 2
                xs_i[0] += 1
                cx.dma("sp", [(xs[xi][:, 0:512], src[r0:r0 + 128, half * 512:(half + 1) * 512])], b_xs[xi], writes=[b_xs[xi]])
                ps, bps = pget("A")
                mm_group(ps[:], bps, [(mergedT[:, k, blk * 128:(blk + 1) * 128], wo[:, k, :]) for k in range(8)],
                         [bwo, b_merged])
                cx.op("dve", lambda xi=xi, ps=ps: dve.tensor_tensor(out=xs[xi][:, 0:512], in0=ps[:], in1=xs[xi][:, 0:512],
                                                                 op=ALU.add),
                      reads=[bps, b_xs[xi]], writes=[b_xs[xi]])
                cx.dma("sp", [(dst[r0:r0 + 128, half * 512:(half + 1) * 512], outs[xi][:, 0:512])], b_outs[xi],
                       reads=[b_outs[xi]], writes=[b_dram[id(dst)][(r0 // 128)]])

    b_dram = {id(scrA): [Buf("scrA%d" % i) for i in range(S // 128)],
              id(scrB): [Buf("scrB%d" % i) for i in range(S // 128)],
              id(out): [Buf("out%d" % i) for i in range(S // 128)],
              id(x_in): [Buf("xin%d" % i) for i in range(S // 128)]}

    def ffn_phase(l, src, dst, moe):
        nexp = NEXP if moe else 1
        F = DEXP if moe else DFF
        nf = F // 128
        if moe:
            cx.dma("sp", [(brB[:], b_router[0:1, :].to_broadcast([128, 8]))], b_brB, writes=[b_brB])
            cx.dma("pool", [(wr[:], w_router[0].rearrange("(k p) e -> p k e", p=128))], b_wr, writes=[b_wr])
        for t in range(S // FT):
            t0 = t * FT
            for blk in range(8):
                r0 = t0 + blk * 128
                cx._deps("sp", [b_dram[id(src)][r0 // 128]], [])
                norm_transpose(src[r0:r0 + 128, :], 2, hTh[blk // 4][:, :, (blk % 4) * 128:(blk % 4 + 1) * 128], b_hTh[blk // 4])
            if moe:
                pl, bpl = pget("X")
                for blk in range(8):
                    mm_group(pl[:, blk * 8:(blk + 1) * 8], bpl,
                             [(hTh[blk // 4][:, k, (blk % 4) * 128:(blk % 4 + 1) * 128], wr[:, k, :]) for k in range(8)],
                             [b_hT, b_QtAll, b_wr])
                v3 = lambda a: a.rearrange("p (b e) -> p b e", b=8)
                bro = lambda a: a.unsqueeze(2).to_broadcast([128, 8, 8])

                b_lg2, b_m1, b_m2 = Buf("lg2"), Buf("m1"), Buf("m2")
                cx.op("dve", lambda: dve.tensor_tensor(out=v3(lg[:]), in0=v3(pl[:, 0:64]),
                                                       in1=brB[:].unsqueeze(1).to_broadcast([128, 8, 8]), op=ALU.add),
                      reads=[bpl, b_brB], writes=[b_lg])
                cx.op("dve", lambda: dve.tensor_reduce(out=m1[:], in_=v3(lg[:]), axis=AX.X, op=ALU.max), reads=[b_lg], writes=[b_m1])
                cx.op("dve", lambda: dve.tensor_tensor(out=v3(lg2[:]), in0=v3(lg[:]), in1=bro(m1[:]), op=ALU.is_equal),
                      reads=[b_lg, b_m1], writes=[b_lg2])
                cx.op("dve", lambda: dve.scalar_tensor_tensor(out=lg2[:], in0=lg2[:], scalar=-1e30, in1=lg[:], op0=ALU.mult, op1=ALU.add),
                      reads=[b_lg, b_lg2], writes=[b_lg2])
                cx.op("dve", lambda: dve.tensor_reduce(out=m2[:], in_=v3(lg2[:]), axis=AX.X, op=ALU.max), reads=[b_lg2], writes=[b_m2])
                cx.op("dve", lambda: dve.tensor_tensor(out=v3(lg2[:]), in0=v3(lg[:]), in1=bro(m2[:]), op=ALU.is_ge),
                      reads=[b_lg, b_m2], writes=[b_lg2])
                cx.op("dve", lambda: dve.tensor_tensor(out=v3(lg[:]), in0=v3(lg[:]), in1=bro(m1[:]), op=ALU.subtract),
                      reads=[b_lg, b_m1], writes=[b_lg])
                cx.op("act", lambda: act.activation(out=lg[:], in_=lg[:], func=AF.Exp), reads=[b_lg], writes=[b_lg])
                cx.op("dve", lambda: dve.tensor_tensor(out=lg[:], in0=lg[:], in1=lg2[:], op=ALU.mult), reads=[b_lg, b_lg2], writes=[b_lg])
                cx.op("dve", lambda: dve.tensor_reduce(out=m1[:], in_=v3(lg[:]), axis=AX.X, op=ALU.add), reads=[b_lg], writes=[b_m1])
                cx.op("dve", lambda: dve.reciprocal(out=m1[:], in_=m1[:]), reads=[b_m1], writes=[b_m1])
                cx.op("dve", lambda: dve.tensor_tensor(out=v3(cw[:]), in0=v3(lg[:]), in1=bro(m1[:]), op=ALU.mult),
                      reads=[b_lg, b_m1], writes=[b_cw])
                pct, bpct = pget("S")
                pct2, bpct2 = pget("S")

                def ftr():
                    for blk in range(8):
                        tgt = pct if blk < 4 else pct2
                        ins = pe.transpose(tgt[0:8, (blk % 4) * 128:(blk % 4 + 1) * 128], cw[:, blk * 8:(blk + 1) * 8], ident_f[:])
                    return ins
                cx.op("pe", ftr, reads=[b_cw, b_const], writes=[bpct, bpct2])
                cx.op("act", lambda: act.copy(out=cwT[0:8, 0:512], in_=pct[0:8, :]), reads=[bpct], writes=[b_cwT])
                cx.op("act", lambda: act.copy(out=cwT[0:8, 512:1024], in_=pct2[0:8, :]), reads=[bpct2], writes=[b_cwT])
            for e in range(nexp):
                wgu = CV["w_ffn_gu"][0].rearrange("(k p) n -> p k n", p=128)
                wdn = CV["w_ffn_down"][0].rearrange("(f p) n -> p f n", p=128)
                cvgu, cvdn = CV["w_ffn_gu"][1], CV["w_ffn_down"][1]
                if moe:
                    ce = e % 2
                    cx.op("dve", lambda e=e: dve.tensor_scalar(out=cwTm[:], in0=cwT[:], scalar1=ident_f[0:8, e:e + 1], scalar2=None,
                                                              op0=ALU.mult), reads=[b_cwT, b_const], writes=[b_cwT])
                    for half in range(2):
                        pcb, bpcb = pget("X")
                        mm_group(pcb[:], bpcb, [(ones_f[0:8, :], cwTm[0:8, half * 512:(half + 1) * 512])], [b_cwT, b_const])
                        cx.op("act", lambda pcb=pcb, half=half, ce=ce: act.copy(out=cwB[ce][:, half * 512:(half + 1) * 512], in_=pcb[:]),
                              reads=[bpcb], writes=[b_cwB[ce]])
                f0 = 0
                while f0 < nf:
                    ng = min(4, nf - f0)
                    wg, bwg = ring_load(wgu[:, :, f0 * 128:(f0 + ng) * 128], (8, ng * 128), cvgu)
                    wu, bwu = ring_load(wgu[:, :, F + f0 * 128:F + (f0 + ng) * 128], (8, ng * 128), cvgu)
                    for fi in range(ng):
                        f = f0 + fi
                        for half in range(2):
                            hs = slice(half * 512, (half + 1) * 512)
                            pg, bpg = pget("A")
                            mm_group(pg[:], bpg, [(wg[:, k, fi * 128:(fi + 1) * 128], hTh[half][:, k, :]) for k in range(8)], [bwg, b_hTh[half]])
                            pu, bpu = pget("O")
                            mm_group(pu[:], bpu, [(wu[:, k, fi * 128:(fi + 1) * 128], hTh[half][:, k, :]) for k in range(8)], [bwu, b_hTh[half]])
                            gi_ = gt_i[0] % 2
                            gt_i[0] += 1
                            cx.op("act", lambda pg=pg, gi_=gi_: act.activation(out=gt[gi_][:], in_=pg[:], func=AF.Silu),
                                  reads=[bpg], writes=[b_gt[gi_]])
                            if moe:
                                cx.op("dve", lambda gi_=gi_, pu=pu: dve.tensor_tensor(out=gt[gi_][:], in0=gt[gi_][:], in1=pu[:], op=ALU.mult),
                                      reads=[b_gt[gi_], bpu], writes=[b_gt[gi_]])
                                cx.op("dve", lambda gi_=gi_, f=f, hs=hs, ce=ce: dve.tensor_tensor(out=aT[:, f, hs], in0=gt[gi_][:],
                                                                                           in1=cwB[ce][:, hs], op=ALU.mult),
                                      reads=[b_gt[gi_], b_cwB[ce]], writes=[b_cache])
                            else:
                                cx.op("dve", lambda gi_=gi_, pu=pu, f=f, hs=hs: dve.tensor_tensor(out=aT[:, f, hs], in0=gt[gi_][:],
                                                                                           in1=pu[:], op=ALU.mult),
                                      reads=[b_gt[gi_], bpu], writes=[b_cache])
                    f0 += ng
                for c in range(8):
                    wd, bwd = ring_load(wdn[:, :, c * 128:(c + 1) * 128], (nf, 128), cvdn)
                    for half in range(2):
                        hs = slice(half * 512, (half + 1) * 512)
                        py, bpy = pget("S")
                        mm_group(py[:], bpy, [(wd[:, f, :], aT[:, f, hs]) for f in range(nf)], [bwd, b_cache])
                        if e == 0:
                            cx.op("act", lambda py=py, c=c, hs=hs: act.copy(out=yT[:, c, hs], in_=py[:]), reads=[bpy], writes=[b_yT])
                        else:
                            cx.op("dve", lambda py=py, c=c, hs=hs: dve.tensor_tensor(out=yT[:, c, hs], in0=yT[:, c, hs], in1=py[:], op=ALU.add),
                                  reads=[bpy, b_yT], writes=[b_yT])
            for blk in range(8):
                r0 = t0 + blk * 128
                xi = xs_i[0] % 2
                xs_i[0] += 1
                cx.dma("sp", [(xs[xi][:], src[r0:r0 + 128, :])], b_xs[xi], reads=[b_dram[id(src)][r0 // 128]], writes=[b_xs[xi]])
                for half in range(2):
                    pt_, bpt = pget("A")

                    def ftr2(pt_=pt_, half=half, blk=blk):
                        for cc in range(4):
                            c = half * 4 + cc
                            ins = pe.transpose(pt_[:, cc * 128:(cc + 1) * 128], yT[:, c, blk * 128:(blk + 1) * 128], ident_f[:])
                        return ins
                    cx.op("pe", ftr2, reads=[b_yT, b_const], writes=[bpt])
                    cx.op("dve", lambda xi=xi, pt_=pt_, half=half: dve.tensor_tensor(
                        out=outs[xi][:, half * 512:(half + 1) * 512], in0=pt_[:], in1=xs[xi][:, half * 512:(half + 1) * 512], op=ALU.add),
                        reads=[bpt, b_xs[xi]], writes=[b_outs[xi]])
                cx.dma("sp", [(dst[r0:r0 + 128, :], outs[xi][:])], b_outs[xi], reads=[b_outs[xi]],
                       writes=[b_dram[id(dst)][r0 // 128]])

    def moe_routed(l, src, dst):
        F = DEXP
        nf = F // 128
        b_HgZ = b_HgZ0
        hg_ev = {}
        yg_ev = {}
        cx.dma("sp", [(brB[:], b_router[0:1, :].to_broadcast([128, 8]))], b_brB, writes=[b_brB])
        cx.dma("pool", [(wr[:], w_router[0].rearrange("(k p) e -> p k e", p=128))], b_wr, writes=[b_wr])
        cx.op("dve", lambda: dve.memset(carry_bc[:], 0.0), writes=[b_cbc])
        lg, lgm = rt[:, 0:8], rt[:, 8:16]
        E2 = rt[:, 16:32].rearrange("p (k e) -> p k e", k=2)
        sel, pos, slotf, valid = rt[:, 32:40], rt[:, 40:48], rt[:, 48:56], rt[:, 56:64]
        prod = rt[:, 64:80].rearrange("p (k e) -> p k e", k=2)
        m1, m2, dd, e2, den = rt[:, 80:81], rt[:, 81:82], rt[:, 82:83], rt[:, 83:84], rt[:, 84:85]
        w12, s12, v12, t12 = rt[:, 85:87], rt[:, 87:89], rt[:, 89:91], rt[:, 91:93]
        R = [b_rt]

        def D_(fn, reads=(), writes=()):
            cx.op("dve", fn, reads=list(reads) + R, writes=list(writes) + R)

        for bi in range(S // 128):
            r0 = bi * 128
            i = xs_i[0] % 2
            xs_i[0] += 1
            xt, bx = xs[i], b_xs[i]
            hb, b_hb = hbs[i], b_hbs[i]
            cx.dma("sp", [(xt[:], src[r0:r0 + 128, :])], bx, reads=[b_dram[id(src)][bi]], writes=[bx])
            cx.op("act", lambda: act.activation(out=hb[:], in_=xt[:], func=AF.Square, accum_out=ss1[:, 0:1]),
                  reads=[bx], writes=[b_hb, b_ss1])
            cx.op("act", lambda: act.activation(out=ss1[:, 1:2], in_=ss1[:, 0:1], func=AF.Sqrt, bias=epsc[:, 0:1],
                                                scale=1.0 / D), reads=[b_ss1, b_const], writes=[b_ss1])
            cx.op("dve", lambda: dve.reciprocal(out=ss1[:, 1:2], in_=ss1[:, 1:2]), reads=[b_ss1], writes=[b_ss1])
            cx.op("act", lambda: act.activation(out=hb[:], in_=xt[:], func=AF.Identity, scale=ss1[:, 1:2]),
                  reads=[bx, b_ss1], writes=[b_hb])
            pt, bp = pget("X")
            ptb = pt.bitcast(BF16)

            def ftp():
                for k in range(8):
                    ins = pe.transpose(ptb[:, k * 128:(k + 1) * 128], hb[:, k * 128:(k + 1) * 128], ident_b[:])
                return ins
            cx.op("pe", ftp, reads=[b_hb, b_const], writes=[bp])
            cx.op("dve", lambda: dve.tensor_tensor(out=hT[:, :, 0:128], in0=ptb[:, 0:1024].rearrange("p (k t) -> p k t", k=8),
                                                   in1=gcols[:, 2, :].unsqueeze(2).to_broadcast([128, 8, 128]), op=ALU.mult),
                  reads=[bp, b_gcols], writes=[b_hT])
            pl, bpl = pget("O")
            mm_group(pl[:, 0:8], bpl, [(hT[:, k, 0:128], wr[:, k, :]) for k in range(8)], [b_hT, b_wr])
            D_(lambda: dve.tensor_tensor(out=lg, in0=pl[:, 0:8], in1=brB[:], op=ALU.add), reads=[bpl, b_brB])
            D_(lambda: dve.tensor_reduce(out=m1, in_=lg, axis=AX.X, op=ALU.max))
            D_(lambda: dve.tensor_tensor(out=E2[:, 0, :], in0=lg, in1=m1.to_broadcast([128, 8]), op=ALU.is_equal))
            D_(lambda: dve.scalar_tensor_tensor(out=lgm, in0=E2[:, 0, :], scalar=-1e30, in1=lg, op0=ALU.mult, op1=ALU.add))
            D_(lambda: dve.tensor_reduce(out=m2, in_=lgm, axis=AX.X, op=ALU.max))
            D_(lambda: dve.tensor_tensor(out=E2[:, 1, :], in0=lgm, in1=m2.to_broadcast([128, 8]), op=ALU.is_equal))
            D_(lambda: dve.tensor_tensor(out=sel, in0=E2[:, 0, :], in1=E2[:, 1, :], op=ALU.add))
            D_(lambda: dve.tensor_tensor(out=dd, in0=m2, in1=m1, op=ALU.subtract))
            cx.op("act", lambda: act.activation(out=e2, in_=dd, func=AF.Exp), reads=R, writes=R)
            D_(lambda: dve.tensor_scalar(out=den, in0=e2, scalar1=1.0, scalar2=None, op0=ALU.add))
            D_(lambda: dve.reciprocal(out=w12[:, 0:1], in_=den))
            D_(lambda: dve.tensor_tensor(out=w12[:, 1:2], in0=e2, in1=w12[:, 0:1], op=ALU.mult))
            pcs, bpcs = pget("S")
            mm_group(pcs[:, 0:8], bpcs, [(triU[:], sel)], R + [b_const])
            ptot, bptot = pget("S")
            mm_group(ptot[:, 0:8], bptot, [(ones_f[:], sel)], R + [b_const])
            D_(lambda: dve.tensor_tensor(out=pos, in0=pcs[:, 0:8], in1=carry_bc[:], op=ALU.add), reads=[bpcs, b_cbc])
            cx.op("dve", lambda: dve.tensor_tensor(out=carry_bc[:], in0=carry_bc[:], in1=ptot[:, 0:8], op=ALU.add),
                  reads=[bptot, b_cbc] + R, writes=[b_cbc])
            D_(lambda: dve.tensor_scalar(out=valid, in0=pos, scalar1=float(CAP), scalar2=None, op0=ALU.is_le))
            D_(lambda: dve.tensor_tensor(out=slotf, in0=pos, in1=eCm1[:], op=ALU.add), reads=[b_const])
            D_(lambda: dve.tensor_tensor(out=prod, in0=E2, in1=slotf.unsqueeze(1).to_broadcast([128, 2, 8]), op=ALU.mult))
            D_(lambda: dve.tensor_reduce(out=s12, in_=prod, axis=AX.X, op=ALU.add))
            D_(lambda: dve.tensor_tensor(out=prod, in0=E2, in1=valid.unsqueeze(1).to_broadcast([128, 2, 8]), op=ALU.mult))
            D_(lambda: dve.tensor_reduce(out=v12, in_=prod, axis=AX.X, op=ALU.add))
            D_(lambda: dve.tensor_tensor(out=t12, in0=s12, in1=v12, op=ALU.mult))
            D_(lambda: dve.tensor_copy(out=gidx[:, bi, :], in_=t12), writes=[b_gidx])
            D_(lambda: dve.tensor_tensor(out=wts[:, bi, :], in0=w12, in1=v12, op=ALU.mult), writes=[b_wts])
            D_(lambda: dve.scalar_tensor_tensor(out=t12, in0=s12, scalar=-BIG, in1=v12, op0=ALU.add, op1=ALU.mult))
            D_(lambda: dve.tensor_scalar(out=t12, in0=t12, scalar1=BIG, scalar2=None, op0=ALU.add))
            si_, bsi = sidx[bi % 2], b_sidx[bi % 2]
            D_(lambda: dve.tensor_copy(out=si_[:], in_=t12), writes=[bsi])
            cx._wait("pool", b_HgZ.writer)
            for k in range(2):
                ev = cx.dma("pool", None, b_hb, reads=[b_hb, bsi], writes=[Buf("hgw")],
                            issue=lambda k=k: pool.indirect_dma_start(
                                out=Hg[:, :], out_offset=bass.IndirectOffsetOnAxis(ap=si_[:, k:k + 1], axis=0),
                                in_=hb[:, :], in_offset=None, bounds_check=bcreg, oob_is_err=False))
                hg_ev[ev[1]] = ev
        MARK('MOE experts')
        segT = [(hT, b_hT), (Qt, b_QtAll), (mergedT, b_merged)]
        for e in range(NEXP):
            wgu = w_exp_gu[0, e].rearrange("(k p) n -> p k n", p=128)
            wdn = w_exp_down[0, e].rearrange("(f p) n -> p f n", p=128)
            MARK('MOE e%d load' % e)
            for b in range(CAP // 128):
                i = xs_i[0] % 2
                xs_i[0] += 1
                hb, b_hb = hbs[i], b_hbs[i]
                for ev in hg_ev.values():
                    cx._wait("sp", ev)
                rr = e * CAP + b * 128
                cx.dma("sp", [(hb[:], Hg[rr:rr + 128, :])], b_hb, writes=[b_hb])
                pt, bp = pget("X")
                ptb = pt.bitcast(BF16)

                def ftp2(hb=hb, ptb=ptb):
                    for k in range(8):
                        ins = pe.transpose(ptb[:, k * 128:(k + 1) * 128], hb[:, k * 128:(k + 1) * 128], ident_b[:])
                    return ins
                cx.op("pe", ftp2, reads=[b_hb, b_const], writes=[bp])
                st, bst = segT[b // 4]
                c0 = (b % 4) * 128
                cx.op("dve", lambda st=st, c0=c0, ptb=ptb: dve.tensor_tensor(
                    out=st[:, :, c0:c0 + 128], in0=ptb[:, 0:1024].rearrange("p (k t) -> p k t", k=8),
                    in1=gcols[:, 2, :].unsqueeze(2).to_broadcast([128, 8, 128]), op=ALU.mult),
                    reads=[bp, b_gcols], writes=[bst])
            MARK('MOE e%d gu' % e)
            f0 = 0
            while f0 < nf:
                ng = min(4, nf - f0)
                wg, bwg = ring_load(wgu[:, :, f0 * 128:(f0 + ng) * 128], (8, ng * 128))
                wu, bwu = ring_load(wgu[:, :, F + f0 * 128:F + (f0 + ng) * 128], (8, ng * 128))
                for fi in range(ng):
                    f = f0 + fi
                    for si2, (s0, n) in enumerate(SEGS):
                        st, bst = segT[si2]
                        pg, bpg = pget("A")
                        mm_group(pg[:, 0:n], bpg, [(wg[:, k, fi * 128:(fi + 1) * 128], st[:, k, 0:n]) for k in range(8)], [bwg, bst])
                        pu, bpu = pget("O")
                        mm_group(pu[:, 0:n], bpu, [(wu[:, k, fi * 128:(fi + 1) * 128], st[:, k, 0:n]) for k in range(8)], [bwu, bst])
                        gi_ = gt_i[0] % 2
                        gt_i[0] += 1
                        cx.op("act", lambda pg=pg, gi_=gi_, n=n: act.activation(out=gt[gi_][:, 0:n], in_=pg[:, 0:n], func=AF.Silu),
                              reads=[bpg], writes=[b_gt[gi_]])
                        cx.op("dve", lambda gi_=gi_, pu=pu, f=f, s0=s0, n=n: dve.tensor_tensor(
                            out=aTr[:, f, s0:s0 + n], in0=gt[gi_][:, 0:n], in1=pu[:, 0:n], op=ALU.mult),
                            reads=[b_gt[gi_], bpu], writes=[b_cache])
                f0 += ng
            MARK('MOE e%d down' % e)
            for chalf in range(2):
                for cc in range(4):
                    c = chalf * 4 + cc
                    wd, bwd = ring_load(wdn[:, :, c * 128:(c + 1) * 128], (nf, 128))
                    for (s0, n) in SEGS:
                        py, bpy = pget("S")
                        mm_group(py[:, 0:n], bpy, [(wd[:, f, :], aTr[:, f, s0:s0 + n]) for f in range(nf)], [bwd, b_cache])
                        cx.op("act", lambda py=py, cc=cc, s0=s0, n=n: act.copy(out=yTs[:, cc, s0:s0 + n], in_=py[:, 0:n]),
                              reads=[bpy], writes=[b_yT])
                for b in range(CAP // 128):
                    i = xs_i[0] % 2
                    xs_i[0] += 1
                    pt_, bpt = pget("A")

                    def ftr3(pt_=pt_, b=b):
                        for cc in range(4):
                            ins = pe.transpose(pt_[:, cc * 128:(cc + 1) * 128], yTs[:, cc, b * 128:(b + 1) * 128], ident_f[:])
                        return ins
                    cx.op("pe", ftr3, reads=[b_yT, b_const], writes=[bpt])
                    cx.op("dve", lambda i=i, pt_=pt_: dve.tensor_copy(out=xs[i][:, 0:512], in_=pt_[:]), reads=[bpt], writes=[b_xs[i]])
                    rr = e * CAP + b * 128
                    ev = cx.dma("sp", [(Yg[rr:rr + 128, chalf * 512:(chalf + 1) * 512], xs[i][:, 0:512])], b_xs[i],
                                reads=[b_xs[i]], writes=[Buf("ygw")])
                    yg_ev[ev[1]] = ev
        MARK('MOE combine')
        ybuf = [brT[:, k, :, :].rearrange("p a t -> p (a t)").bitcast(F32) for k in range(2)]
        for bi in range(S // 128):
            r0 = bi * 128
            i = xs_i[0] % 2
            xs_i[0] += 1
            cx.dma("sp", [(xs[i][:], src[r0:r0 + 128, :])], b_xs[i], reads=[b_dram[id(src)][bi]], writes=[b_xs[i]])
            for k in range(2):
                for ev in yg_ev.values():
                    cx._wait("pool", ev)
                bb = b_brT[k]
                cx.dma("pool", None, bb, reads=[b_gidx], writes=[bb],
                       issue=lambda k=k, bi=bi: pool.indirect_dma_start(
                           out=ybuf[k][:, :], out_offset=None, in_=Yg[:, :],
                           in_offset=bass.IndirectOffsetOnAxis(ap=gidx[:, bi, k:k + 1], axis=0),
                           bounds_check=bcreg, oob_is_err=False))
                cx.op("dve", lambda i=i, k=k, bi=bi: dve.scalar_tensor_tensor(
                    out=xs[i][:], in0=ybuf[k][:], scalar=wts[:, bi, k:k + 1], in1=xs[i][:], op0=ALU.mult, op1=ALU.add),
                    reads=[bb, b_wts, b_xs[i]], writes=[b_xs[i]])
            cx.dma("sp", [(dst[r0:r0 + 128, :], xs[i][:])], b_xs[i], reads=[b_xs[i]], writes=[b_dram[id(dst)][bi]])

    b_yT = Buf("yT")

    def phase_barrier(to_ffn):
        allb = [b for hb_ in b_Kt for b in hb_] + b_Vc
        if to_ffn:
            for e in ("dve", "act", "pe"):
                cx._deps(e, [], allb)
        else:
            for e in ("dve", "act", "pe", "pool"):
                cx._deps(e, [], [b_cache, b_yT])

    for l_ in range(DEPTH):
        convert("w_mem_kv%d" % l_, w_mem_kv[l_])
        convert("w_in%d" % l_, w_in[l_])
        for br_, wsrc_ in enumerate((w_pool_br, w_fox_br, w_mem_br)):
            convert("w_br%d_%d" % (br_, l_), wsrc_[l_])
        convert("w_out%d" % l_, w_out[l_])
        if l_ == 0:
            convert("w_ffn_gu", w_ffn_gu[0])
            convert("w_ffn_down", w_ffn_down[0])
    b_HgZ0 = Buf("HgZ0")
    cx.dma("sp", [(Hg[bz * 128:(bz + 1) * 128, :], hbs[0][:]) for bz in range(NSL // 128)], b_hbs[0],
           reads=[b_hbs[0]], writes=[b_HgZ0])
    chain = [(x_in, scrA), (scrA, scrB), (scrB, scrA), (scrA, out)]
    for l in range(dbg.get("layers", DEPTH)):
        load_layer_consts(l)
        mem_kv(l)
        src, dst = chain[2 * l]
        if l > 0:
            cx.op("pool", lambda: pool.memset(halo[:], 0.0), writes=[b_halo])
            cx.op("pool", lambda: pool.memset(carry[:], 0.0), writes=[b_carry])
            cx.op("pool", lambda: pool.memset(Vc[:, :, :, 64:65], 1.0), writes=b_Vc)
        for i in range(dbg.get("tiles", NT)):
            for blk in range(4):
                cx._deps("sp", [b_dram[id(src)][(i * TT) // 128 + blk]], [])
            mixer_tile(l, i, src, dst)
        if dbg.get("dump_mixer") and l == dbg.get("layers", DEPTH) - 1:
            break
        MARK('L%d FFN' % l)
        phase_barrier(True)
        src, dst = chain[2 * l + 1]
        if l % 2 == 1:
            moe_routed(l, src, dst)
        else:
            ffn_phase(l, src, dst, moe=False)
        phase_barrier(False)
    if dbg.get("dump_mixer"):
        dumps = {"d_hT": (hT, [b_hT]), "d_Qt": (Qt, [b_QtAll]), "d_cache": (cache, [b for hb_ in b_Kt for b in hb_] + b_Vc),
                 "d_brT": (brT, b_brT), "d_merged": (mergedT, [b_merged]), "d_Cs": (Cs, [b_Cs]), "d_PIE": (PIE, [b_PIE]),
                 "d_KmT": (KmT, [b_KmT]), "d_Vm": (Vm, [b_Vm]), "d_cols": (cols, [b_cols]), "d_nl": (nl, [b_nl]),
                 "d_invc": (invc, [b_const]), "d_tmpf": (tmpf, [b_tmpf]), "d_U": (U, [b_U]),
                 "d_pT0": (pT[0], [b_pT[0]]), "d_pT1": (pT[1], [b_pT[1]]), "d_pT2": (pT[2], [b_pT[2]]), "d_pT3": (pT[3], [b_pT[3]]),
                 "d_La": (La, [b_La]), "d_Lb": (Lb, [b_Lb])}
        if dbg.get("nodump"):
            dumps = {}
        for nm, (ap_, bufs) in dumps.items():
            shp = list(ap_.shape)
            dt_ = ap_.dtype
            o_ = nc.dram_tensor(nm, shp, dt_, kind="ExternalOutput").ap()
            cx.dma("sp", [(o_, ap_)], bufs[0], reads=bufs, writes=[Buf("dd_" + nm)])
            cx.finish([bufs[0]])
        for b_ in b_dram[id(scrA)] + b_dram[id(scrB)]:
            cx.finish([b_])
    cx.finish(b_dram[id(out)])
    MARK('END')
    nc._marks = MARKS
    return nc


_NC_CACHE = {}


def kernel(**inputs):
    if "nc" not in _NC_CACHE:
        _NC_CACHE["nc"] = build_program()
    nc = _NC_CACHE["nc"]
    names = ["mix_norm_g", "w_in", "b_forget", "fox_q_g", "fox_k_g", "pool_w", "pool_scale", "mem_norm_g",
             "w_mem_kv", "mem_q_g", "mem_k_g", "w_pool_br", "w_fox_br", "w_mem_br", "w_out", "ffn_norm_g",
             "w_ffn_gu", "w_ffn_down", "w_router", "b_router", "w_exp_gu", "w_exp_down"]
    shared = {n: np.ascontiguousarray(np.asarray(inputs[n], dtype=np.float32)) for n in names}
    x = np.asarray(inputs["x"], dtype=np.float32)
    mem = np.asarray(inputs["mem"], dtype=np.float32)
    in_maps = []
    for b in range(8):
        m = dict(shared)
        m["x"] = np.ascontiguousarray(x[b])
        m["mem"] = np.ascontiguousarray(mem[b])
        in_maps.append(m)
    res = run_bass_kernel_spmd(nc, in_maps, core_ids=list(range(8)))
    return np.stack([np.asarray(res.results[b]["out"]) for b in range(8)], axis=0).astype(np.float32)
```

```python
import numpy as np
import concourse.bass as bass
import concourse.mybir as mybir
from concourse.bass_utils import run_bass_kernel_spmd

F32 = mybir.dt.float32
BF16 = mybir.dt.bfloat16
I32 = mybir.dt.int32
AF = mybir.ActivationFunctionType
ALU = mybir.AluOpType
AX = mybir.AxisListType

D = 1024
S = 4096
DEPTH = 2
NMEM = 256
NIN = 5640
OFF_POOL, OFF_Q, OFF_K, OFF_V, OFF_F, OFF_MQ, OFF_G = 0, 512, 1024, 1536, 2048, 2056, 2568
DFF = 2816
NEXP = 8
DEXP = 3584
EPS = 1e-6
TT = 512
NT = S // TT
FT = 1024
SLOT_COLS = 4096
NSLOT = 3
CAP = 1280
NSL = NEXP * CAP
SEGS = [(0, 512), (512, 512), (1024, 256)]
BIG = 1.0e6


class Buf:
    __slots__ = ("name", "writer", "readers", "dsems")

    def __init__(self, name):
        self.name = name
        self.writer = None
        self.readers = {}
        self.dsems = {}


class Ctx:
    def __init__(self, nc):
        self.nc = nc
        self.eng = {"pe": nc.tensor, "act": nc.scalar, "dve": nc.vector, "pool": nc.gpsimd, "sp": nc.sync}
        self.sem = {k: nc.alloc_semaphore("s_" + k) for k in self.eng}
        self.cnt = {k: 0 for k in self.eng}
        self.waited = {k: {} for k in self.eng}
        self.nsem = 0

    def _wait(self, e, ev, raw=True):
        if ev is None:
            return
        kind, key, val, sem = ev
        if kind == "eng" and key == e and (e == "pe" or not raw):
            return
        w = self.waited[e]
        if w.get(key, 0) >= val:
            return
        w[key] = val
        self.eng[e].wait_ge(sem, val)

    @staticmethod
    def _flat(bufs):
        out = []
        for b in bufs:
            if isinstance(b, (list, tuple)):
                out.extend(Ctx._flat(b))
            else:
                out.append(b)
        return out

    def _deps(self, e, reads, writes):
        reads, writes = self._flat(reads), self._flat(writes)
        for b in reads:
            self._wait(e, b.writer)
        for b in writes:
            self._wait(e, b.writer, raw=False)
            for ev in b.readers.values():
                self._wait(e, ev, raw=False)

    def _record(self, ev, rkey, reads, writes):
        reads, writes = self._flat(reads), self._flat(writes)
        for b in reads:
            b.readers[rkey] = ev
        for b in writes:
            b.writer = ev
            b.readers = {}

    def op(self, e, fn, reads=(), writes=()):
        self._deps(e, reads, writes)
        ins = fn()
        self.cnt[e] += 1
        ins.then_inc(self.sem[e], 1)
        ev = ("eng", e, self.cnt[e], self.sem[e])
        self._record(ev, e, reads, writes)

    def dma(self, q, pairs, sb, reads=(), writes=(), issue=None):
        self._deps(q, reads, writes)
        kind = "sw" if q == "pool" else "hw"
        st = sb.dsems.setdefault(kind, [None, 0])
        if st[0] is None:
            self.nsem += 1
            st[0] = self.nc.alloc_semaphore("d%d" % self.nsem)
        if issue is not None:
            ins = issue()
            st[1] += 16
            ins.then_inc(st[0], 16)
        else:
            for (o, i) in pairs:
                ins = self.eng[q].dma_start(out=o, in_=i)
                st[1] += 16
                ins.then_inc(st[0], 16)
        key = "d%s_%s" % (kind, sb.name)
        ev = ("dma", key, st[1], st[0])
        self._record(ev, key, reads, writes)
        return ev

    def finish(self, bufs):
        for b in bufs:
            self._wait("sp", b.writer)
            for ev in b.readers.values():
                self._wait("sp", ev)


def build_program(dbg=None):
    dbg = dbg or {}
    nc = bass.Bass("TRN2", target_bir_lowering=False)
    cx = Ctx(nc)
    pe, act, dve, pool = nc.tensor, nc.scalar, nc.vector, nc.gpsimd

    def din(name, shape):
        return nc.dram_tensor(name, list(shape), F32, kind="ExternalInput").ap()

    x_in = din("x", [S, D])
    mem_in = din("mem", [NMEM, D])
    mix_norm_g = din("mix_norm_g", [DEPTH, D])
    w_in = din("w_in", [DEPTH, D, NIN])
    b_forget = din("b_forget", [DEPTH, 8])
    fox_q_g = din("fox_q_g", [DEPTH, 64])
    fox_k_g = din("fox_k_g", [DEPTH, 64])
    pool_w = din("pool_w", [DEPTH, 4, 128, 128])
    pool_scale = din("pool_scale", [DEPTH, 512])
    mem_norm_g = din("mem_norm_g", [DEPTH, D])
    w_mem_kv = din("w_mem_kv", [DEPTH, D, 1024])
    mem_q_g = din("mem_q_g", [DEPTH, 128])
    mem_k_g = din("mem_k_g", [DEPTH, 128])
    w_pool_br = din("w_pool_br", [DEPTH, 512, D])
    w_fox_br = din("w_fox_br", [DEPTH, 512, D])
    w_mem_br = din("w_mem_br", [DEPTH, 512, D])
    w_out = din("w_out", [DEPTH, D, D])
    ffn_norm_g = din("ffn_norm_g", [DEPTH, D])
    w_ffn_gu = din("w_ffn_gu", [1, D, 2 * DFF])
    w_ffn_down = din("w_ffn_down", [1, DFF, D])
    w_router = din("w_router", [1, D, NEXP])
    b_router = din("b_router", [1, NEXP])
    w_exp_gu = din("w_exp_gu", [1, NEXP, D, 2 * DEXP])
    w_exp_down = din("w_exp_down", [1, NEXP, DEXP, D])
    out = nc.dram_tensor("out", [S, D], F32, kind="ExternalOutput").ap()
    skind = "ExternalOutput" if dbg else "Internal"
    scrA = nc.dram_tensor("scrA", [S, D], F32, kind=skind).ap()
    scrB = nc.dram_tensor("scrB", [S, D], F32, kind=skind).ap()
    Hg = nc.dram_tensor("Hg", [NSL, D], BF16, kind="Internal").ap()
    Yg = nc.dram_tensor("Yg", [NSL, D], F32, kind="Internal").ap()

    def sb(name, shape, dt):
        return nc.alloc_sbuf_tensor(name, list(shape), dt).ap()

    CACHE_COLS = 8 * S + 32 * 8 * 65
    cache = sb("cache", [128, CACHE_COLS], BF16)
    Kt = [cache[:, h * S:(h + 1) * S] for h in range(8)]
    Vc = cache[:, 8 * S:8 * S + 32 * 520].rearrange("p (b h e) -> p b h e", b=32, h=8)
    aT = cache[:, 0:28 * FT].rearrange("p (f t) -> p f t", f=28)
    yT = cache[:, 28 * FT:28 * FT + 2 * 8 * FT].bitcast(F32).rearrange("p (c t) -> p c t", c=8)
    b_cache = Buf("cache_all")
    aTr = cache[:, 0:28 * CAP].rearrange("p (f t) -> p f t", f=28)
    yTs = cache[:, 28 * CAP:28 * CAP + 2 * 4 * CAP].bitcast(F32).rearrange("p (c t) -> p c t", c=4)

    ring = [sb("ring%d" % i, [128, SLOT_COLS], BF16) for i in range(NSLOT)]
    b_ring = [Buf("ring%d" % i) for i in range(NSLOT)]
    ring_i = [0]

    hT = sb("hT", [128, 8, TT], BF16)
    b_hT = Buf("hT")
    Qt = sb("Qt", [128, 8, TT], BF16)
    b_QtAll = Buf("Qt")
    b_Qt = [b_QtAll for h in range(8)]
    hTh = [hT, Qt]
    b_hTh = [b_hT, b_QtAll]
    b_Kt = [[Buf("Kt%d_%d" % (h, i)) for i in range(NT)] for h in range(8)]
    b_Vc = [Buf("Vc%d" % i) for i in range(NT)]
    xs = [sb("xs%d" % i, [128, D], F32) for i in range(2)]
    b_xs = [Buf("xs%d" % i) for i in range(2)]
    hbs = [sb("hb%d" % i, [128, D], BF16) for i in range(2)]
    b_hbs = [Buf("hb%d" % i) for i in range(2)]
    outs = xs
    b_outs = b_xs
    gcols = sb("gcols", [128, 3, 8], F32)
    b_gcols = Buf("gcols")
    brT = sb("brT", [128, 3, 4, TT], BF16)
    b_brT = [Buf("brT%d" % i) for i in range(3)]
    mergedT = sb("mergedT", [128, 8, TT], BF16)
    b_merged = Buf("merged")
    efs = [mergedT[:, k, :] for k in range(6)]
    b_efs = [Buf("efs%d" % k) for k in range(6)]
    efs_i = [0]

    def ef_next():
        k = efs_i[0] % 6
        efs_i[0] += 1
        return efs[k], b_efs[k]
    macc = sb("macc", [128, 2, TT], F32)
    b_maccAll = Buf("macc")
    b_macc = [b_maccAll for i in range(4)]
    NPT = 4
    pT = [sb("pT%d" % i, [128, TT], BF16) for i in range(NPT)]
    b_pT = [Buf("pT%d" % i) for i in range(NPT)]
    pT_i = [0]

    def pt_next():
        k = pT_i[0] % NPT
        pT_i[0] += 1
        return k
    rec = sb("rec", [128, TT], F32)
    b_rec = Buf("rec")
    rs, b_rs = rec, b_rec
    gt = [sb("gt%d" % i, [128, TT], F32) for i in range(2)]
    b_gt = [Buf("gt%d" % i) for i in range(2)]
    gt_i = [0]
    tmpf = sb("tmpf", [128, 16], F32)
    b_tmpf = Buf("tmpf")
    U = sb("U", [128, 16 + TT], F32)
    b_U = Buf("U")
    La = sb("La", [128, 16 + TT], F32)
    b_La = Buf("La")
    Lb = sb("Lb", [128, 16 + TT], F32)
    b_Lb = Buf("Lb")
    halo = sb("halo", [128, 4, 16], F32)
    b_halo = Buf("halo")
    tf = sb("tf", [128, 32], F32)
    b_tf = Buf("tf")
    nl = sb("nl", [128, 32], F32)
    b_nl = Buf("nl")
    Cs = sb("Cs", [8, TT], F32)
    b_Cs = Buf("Cs")
    carry = sb("carry", [8, 1], F32)
    b_carry = Buf("carry")
    r1 = sb("r1", [8, TT], F32)
    b_r1 = Buf("r1")
    lot = sb("lot", [8, TT], BF16)
    b_lot = Buf("lot")
    PIE = sb("PIE", [128, TT], BF16)
    b_PIE = Buf("PIE")
    memT = macc[:, 0:2, :].rearrange("p a t -> p (a t)").bitcast(BF16).rearrange("p (k m) -> p k m", k=8)
    b_memT = b_maccAll
    KmT = sb("KmT", [128, 4, NMEM], BF16)
    b_KmT = Buf("KmT")
    Vm = sb("Vm", [128, 2, 512], BF16)
    b_Vm = Buf("Vm")
    poolw = sb("poolw", [128, 4, 128], BF16)
    b_poolw = Buf("poolw")
    cols = sb("cols", [128, 16], F32)
    b_cols = Buf("cols")
    bfB = sb("bfB", [128, 8], F32)
    b_bfB = Buf("bfB")
    brB = sb("brB", [128, 8], F32)
    b_brB = Buf("brB")
    cwB = [brT[:, i, :, :].rearrange("p a t -> p (a t)").bitcast(F32) for i in range(2)]
    b_cwB = [b_brT[i] for i in range(2)]
    cwT = mergedT[0:8, 0:4, :].rearrange("p a t -> p (a t)").bitcast(F32)
    cwTm = mergedT[0:8, 4:8, :].rearrange("p a t -> p (a t)").bitcast(F32)
    b_cwT = b_merged
    rt = sb("rt", [128, 96], F32)
    b_rt = Buf("rt")
    gidx = sb("gidx", [128, 32, 2], I32)
    b_gidx = Buf("gidx")
    sidx = [sb("sidx%d" % i, [128, 2], I32) for i in range(2)]
    b_sidx = [Buf("sidx%d" % i) for i in range(2)]
    wts = sb("wts", [128, 32, 2], F32)
    b_wts = Buf("wts")
    carry_bc = sb("carry_bc", [128, 8], F32)
    b_cbc = Buf("carry_bc")
    eCm1 = sb("eCm1", [128, 8], F32)
    wf = sb("wf", [128, 8, 8], BF16)
    b_wf = Buf("wf")
    wr = sb("wr", [128, 8, 8], BF16)
    b_wr = Buf("wr")
    ss1 = sb("ss1", [128, 2], F32)
    b_ss1 = Buf("ss1")
    ident_b = sb("ident_b", [128, 128], BF16)
    ident_f = sb("ident_f", [128, 128], F32)
    blk64 = sb("blk64", [128, 128], BF16)
    ones_b = sb("ones_b", [128, 128], BF16)
    ones_f = sb("ones_f", [128, 128], F32)
    triU = sb("triU", [128, 128], F32)
    maskT = sb("maskT", [128, 128], BF16)
    SelK = sb("SelK", [128, 8, 70], BF16)
    SelQ = sb("SelQ", [128, 8, 70], BF16)
    invc = sb("invc", [128, 4, 16], F32)
    epsc = sb("epsc", [128, 1], F32)
    b_const = Buf("const")

    pbank = [nc.alloc_psum_tensor("pb%d" % i, [128, 512], F32).ap() for i in range(8)]
    b_pb = [Buf("pb%d" % i) for i in range(8)]
    pp = {"A": [0, 1, 2], "S": [3, 4], "O": [5, 6], "X": [7], "SA": [3, 4, 0], "EF": [1, 2]}
    pp_i = {k: 0 for k in pp}

    def pget(kind):
        lst = pp[kind]
        i = lst[pp_i[kind] % len(lst)]
        pp_i[kind] += 1
        return pbank[i], b_pb[i]

    MARKS = []

    def MARK(name):
        MARKS.append((name, dict(cx.cnt)))

    def setup_consts():
        def P(fn, bufs=None):
            cx.op("pool", fn, reads=bufs or [b_const], writes=bufs or [b_const])

        def eye(t):
            P(lambda: pool.memset(t[:], 1.0))
            P(lambda: pool.affine_select(out=t[:], in_=t[:], pattern=[[-1, 128]], compare_op=ALU.is_equal,
                                         fill=0.0, base=0, channel_multiplier=1))
        eye(ident_b)
        eye(ident_f)
        P(lambda: pool.memset(ones_b[:], 1.0))
        P(lambda: pool.memset(ones_f[:], 1.0))
        P(lambda: pool.memset(blk64[:], 0.0))
        P(lambda: pool.memset(blk64[0:64, 0:64], 1.0))
        P(lambda: pool.memset(blk64[64:128, 64:128], 1.0))
        P(lambda: pool.memset(triU[:], 1.0))
        P(lambda: pool.affine_select(out=triU[:], in_=triU[:], pattern=[[1, 128]], compare_op=ALU.is_ge,
                                     fill=0.0, base=0, channel_multiplier=-1))
        P(lambda: pool.memset(maskT[:], 0.0))
        P(lambda: pool.affine_select(out=maskT[:], in_=maskT[:], pattern=[[1, 128]], compare_op=ALU.is_ge,
                                     fill=-30000.0, base=0, channel_multiplier=-1))
        P(lambda: pool.memset(SelK[:], 1.0))
        P(lambda: pool.memset(SelK[:, :, 0:64], 0.0))
        for j in range(3):
            P(lambda j=j: pool.affine_select(out=SelK[:, :, 64 + j:65 + j], in_=SelK[:, :, 64 + j:65 + j],
                                             pattern=[[-1, 8], [0, 1]], compare_op=ALU.is_equal, fill=0.0,
                                             base=-32 * j, channel_multiplier=1))
        P(lambda: pool.affine_select(out=SelK[:, :, 67:70], in_=SelK[:, :, 67:70], pattern=[[0, 8], [0, 3]],
                                     compare_op=ALU.is_equal, fill=0.0, base=-96, channel_multiplier=1))
        P(lambda: pool.memset(SelQ[:], 1.0))
        P(lambda: pool.memset(SelQ[:, :, 0:64], 0.0))
        P(lambda: pool.memset(SelQ[:, :, 67:70], -1.0))
        P(lambda: pool.affine_select(out=SelQ[:, :, 64:67], in_=SelQ[:, :, 64:67], pattern=[[0, 8], [0, 3]],
                                     compare_op=ALU.is_equal, fill=0.0, base=-96, channel_multiplier=1))
        for j in range(3):
            P(lambda j=j: pool.affine_select(out=SelQ[:, :, 67 + j:68 + j], in_=SelQ[:, :, 67 + j:68 + j],
                                             pattern=[[-1, 8], [0, 1]], compare_op=ALU.is_equal, fill=0.0,
                                             base=-32 * j, channel_multiplier=1))
        for g in range(4):
            w = 2 ** (g + 1)
            P(lambda g=g: pool.memset(invc[:, g, :], 1.0))
            for t in range(min(w - 1, 16)):
                P(lambda g=g, t=t, w=w: pool.memset(invc[:, g, t:t + 1], float(w) / (t + 1)))
        P(lambda: pool.memset(epsc[:], EPS))
        for e_ in range(NEXP):
            P(lambda e_=e_: pool.memset(eCm1[:, e_:e_ + 1], float(e_ * CAP - 1)))
        P(lambda: pool.memset(hbs[0][:], 0.0), [b_hbs[0]])
        P(lambda: pool.memset(PIE[:], 0.0), [b_PIE])
        P(lambda: pool.memset(PIE[96:97, :], 1.0), [b_PIE])
        P(lambda: pool.memset(halo[:], 0.0), [b_halo])
        P(lambda: pool.memset(carry[:], 0.0), [b_carry])
        P(lambda: pool.memset(Vc[:, :, :, 64:65], 1.0), b_Vc)

    setup_consts()
    bcreg = nc.gpsimd.alloc_register("bcreg")
    nc.gpsimd.reg_mov(bcreg, NSL - 1)

    b_half = [Buf("ringh%d" % i) for i in range(2 * NSLOT)]
    HALF = SLOT_COLS // 2

    def ring_load(src_ap, shape3, cv=None):
        a, b = shape3
        n_el = a * b
        if n_el <= HALF:
            h = ring_i[0] % (2 * NSLOT)
            ring_i[0] += 1
            view = ring[h // 2][:, (h % 2) * HALF:(h % 2) * HALF + n_el].rearrange("p (a b) -> p a b", a=a)
            bufs = [b_half[h]]
        else:
            if ring_i[0] % 2:
                ring_i[0] += 1
            h = ring_i[0] % (2 * NSLOT)
            ring_i[0] += 2
            view = ring[h // 2][:, 0:n_el].rearrange("p (a b) -> p a b", a=a)
            bufs = [b_half[h], b_half[h + 1]]
        if cv is None:
            cx.dma("pool", [(view, src_ap)], bufs[0], writes=bufs)
        else:
            cx.dma("sp", [(view, src_ap)], bufs[0], reads=[cv], writes=bufs)
        return view, bufs

    CV = {}

    def convert(name, src2d):
        R_, C_ = src2d.shape
        dstc = nc.dram_tensor("bf_" + name, [R_, C_], BF16, kind="Internal").ap()
        bcv = Buf("cv_" + name)
        cx.dma("pool", [(dstc[r:r + 128, :], src2d[r:r + 128, :]) for r in range(0, R_, 128)], bcv, writes=[bcv])
        CV[name] = (dstc, bcv)

    def load_layer_consts(l):
        gprs = []
        for gi2, gsrc in enumerate((mix_norm_g, mem_norm_g, ffn_norm_g)):
            for k in range(8):
                gprs.append((gcols[:, gi2, k:k + 1], gsrc[l, k * 128:(k + 1) * 128].rearrange("(p o) -> p o", o=1)))
        cx.dma("sp", gprs, b_gcols, writes=[b_gcols])
        cx.dma("sp", [(bfB[:], b_forget[l:l + 1, :].to_broadcast([128, 8]))], b_bfB, writes=[b_bfB])
        prs = []
        qg = fox_q_g[l].rearrange("(p o) -> p o", o=1)
        kg = fox_k_g[l].rearrange("(p o) -> p o", o=1)
        for j in range(2):
            prs.append((cols[j * 64:(j + 1) * 64, 0:1], qg))
            prs.append((cols[j * 64:(j + 1) * 64, 1:2], kg))
        prs.append((cols[:, 2:3], mem_q_g[l].rearrange("(p o) -> p o", o=1)))
        prs.append((cols[:, 3:4], mem_k_g[l].rearrange("(p o) -> p o", o=1)))
        for g in range(4):
            prs.append((cols[:, 4 + g:5 + g], pool_scale[l, g * 128:(g + 1) * 128].rearrange("(p o) -> p o", o=1)))
        cx.dma("sp", prs, b_cols, writes=[b_cols])
        cx.op("dve", lambda: dve.tensor_scalar(out=cols[:, 0:1], in0=cols[:, 0:1], scalar1=0.125, scalar2=None,
                                               op0=ALU.mult), reads=[b_cols], writes=[b_cols])
        cx.op("dve", lambda: dve.tensor_scalar(out=cols[:, 2:3], in0=cols[:, 2:3], scalar1=float(128 ** -0.5),
                                               scalar2=None, op0=ALU.mult), reads=[b_cols], writes=[b_cols])
        cx.dma("pool", [(poolw[:], pool_w[l].rearrange("g p n -> p g n"))], b_poolw, writes=[b_poolw])

    xs_i = [0]

    def norm_transpose(src_rows, gsel, dstT, b_dst):
        i = xs_i[0] % 2
        xs_i[0] += 1
        xt, bx = xs[i], b_xs[i]
        hb, b_hb = hbs[i], b_hbs[i]
        cx.dma("sp", [(xt[:], src_rows)], bx, writes=[bx])
        cx.op("act", lambda: act.activation(out=hb[:], in_=xt[:], func=AF.Square, accum_out=ss1[:, 0:1]),
              reads=[bx], writes=[b_hb, b_ss1])
        cx.op("act", lambda: act.activation(out=ss1[:, 1:2], in_=ss1[:, 0:1], func=AF.Sqrt, bias=epsc[:, 0:1],
                                            scale=1.0 / D), reads=[b_ss1, b_const], writes=[b_ss1])
        cx.op("dve", lambda: dve.reciprocal(out=ss1[:, 1:2], in_=ss1[:, 1:2]), reads=[b_ss1], writes=[b_ss1])
        cx.op("act", lambda: act.activation(out=hb[:], in_=xt[:], func=AF.Identity, scale=ss1[:, 1:2]),
              reads=[bx, b_ss1], writes=[b_hb])
        pt, bp = pget("X")
        ptb = pt.bitcast(BF16)

        def f():
            for k in range(8):
                ins = pe.transpose(ptb[:, k * 128:(k + 1) * 128], hb[:, k * 128:(k + 1) * 128], ident_b[:])
            return ins
        cx.op("pe", f, reads=[b_hb, b_const], writes=[bp])
        cx.op("dve", lambda: dve.tensor_tensor(out=dstT, in0=ptb[:, 0:1024].rearrange("p (k t) -> p k t", k=8),
                                               in1=gcols[:, gsel, :].unsqueeze(2).to_broadcast([128, 8, 128]), op=ALU.mult),
              reads=[bp, b_gcols], writes=[b_dst])

    def pipelined(items, produce, consume, la=1):
        outs_ = {}
        n_ = len(items)
        for t_ in range(n_ + la):
            if t_ < n_:
                outs_[t_] = produce(items[t_])
            if t_ - la >= 0:
                consume(items[t_ - la], outs_.pop(t_ - la))

    def mm_group(out_ap, b_out, pairs, reads, first=True, last=True):
        def f():
            n = len(pairs)
            for idx, (l, r) in enumerate(pairs):
                ins = pe.matmul(out_ap, lhsT=l, rhs=r, start=(first and idx == 0), stop=(last and idx == n - 1))
            return ins
        cx.op("pe", f, reads=reads, writes=[b_out])

    def head_norm(ps, bps, ones_mat, inv_n, gcol_idx, dsts, b_dsts, n=TT):
        kq = pt_next()
        sqs, b_sqs = pT[kq], b_pT[kq]
        cx.op("act", lambda: act.activation(out=sqs[:, 0:n], in_=ps[:, 0:n], func=AF.Square), reads=[bps], writes=[b_sqs])
        p2, bp2 = pget("X")
        mm_group(p2[:, 0:n], bp2, [(ones_mat[:], sqs[:, 0:n])], [b_sqs, b_const])
        cx.op("act", lambda: act.activation(out=rs[:, 0:n], in_=p2[:, 0:n], func=AF.Sqrt, bias=epsc[:, 0:1], scale=inv_n),
              reads=[bp2, b_const], writes=[b_rs])
        cx.op("dve", lambda: dve.reciprocal(out=rs[:, 0:n], in_=rs[:, 0:n]), reads=[b_rs], writes=[b_rs])
        for (lo, hi, o), bd in zip(dsts, b_dsts):
            cx.op("dve", lambda lo=lo, hi=hi, o=o: dve.scalar_tensor_tensor(
                out=o, in0=ps[lo:hi, 0:n], scalar=cols[lo:hi, gcol_idx:gcol_idx + 1], in1=rs[lo:hi, 0:n],
                op0=ALU.mult, op1=ALU.mult), reads=[bps, b_rs, b_cols], writes=[bd])

    def mem_kv(l):
        for blk in range(2):
            norm_transpose(mem_in[blk * 128:(blk + 1) * 128, :], 1,
                           memT[:, :, blk * 128:(blk + 1) * 128], b_memT)
        wkv_src, wkv_cv = CV["w_mem_kv%d" % l]
        wk, bwk = ring_load(wkv_src.rearrange("(k p) n -> p k n", p=128)[:, :, 0:512], (8, 512), wkv_cv)
        for hm in range(4):
            ps, bps = pget("A")
            mm_group(ps[:, 0:NMEM], bps, [(wk[:, k, hm * 128:(hm + 1) * 128], memT[:, k, :]) for k in range(8)],
                     [bwk, b_memT])
            head_norm(ps, bps, ones_b, 1.0 / 128, 3, [(0, 128, KmT[:, hm, :])], [b_KmT], n=NMEM)
        wv, bwv = ring_load(wkv_src.rearrange("(k p) n -> p k n", p=128)[:, :, 512:1024], (8, 512), wkv_cv)
        for blk in range(2):
            ps, bps = pget("A")
            mm_group(ps[:], bps, [(memT[:, k, blk * 128:(blk + 1) * 128], wv[:, k, :]) for k in range(8)],
                     [bwv, b_memT])
            cx.op("act", lambda blk=blk, ps=ps: act.copy(out=Vm[:, blk, :], in_=ps[:]), reads=[bps], writes=[b_Vm])

    def mixer_tile(l, i, src, dst):
        t0 = i * TT
        MARK('L%d T%d A' % (l, i))
        win = CV["w_in%d" % l][0].rearrange("(k p) n -> p k n", p=128)
        cvin = CV["w_in%d" % l][1]
        for blk in range(4):
            norm_transpose(src[t0 + blk * 128:t0 + (blk + 1) * 128, :], 0,
                           hT[:, :, blk * 128:(blk + 1) * 128], b_hT)
        MARK('L%d T%d B' % (l, i))
        itemsB = [(off, gi, is_q, c) for (off, gi, is_q) in ((OFF_K, 1, False), (OFF_Q, 0, True)) for c in range(4)]
        wcur = {}

        def prodB(it):
            off, gi, is_q, c = it
            if c == 0:
                wcur["w"] = ring_load(win[:, :, off:off + 512], (8, 512), cvin)
            w, bw = wcur["w"]
            ps, bps = pget("A")
            mm_group(ps[:], bps, [(w[:, k, c * 128:(c + 1) * 128], hT[:, k, 0:TT]) for k in range(8)], [bw, b_hT])
            return ps, bps

        def consB(it, o):
            off, gi, is_q, c = it
            ps, bps = o
            if is_q:
                dsts = [(j * 64, (j + 1) * 64, Qt[0:64, 2 * c + j, :]) for j in range(2)]
                bds = [b_Qt[2 * c + j] for j in range(2)]
            else:
                dsts = [(j * 64, (j + 1) * 64, Kt[2 * c + j][0:64, t0:t0 + TT]) for j in range(2)]
                bds = [b_Kt[2 * c + j][i] for j in range(2)]
            head_norm(ps, bps, blk64, 1.0 / 64, gi, dsts, bds)
        pipelined(itemsB, prodB, consB)
        MARK('L%d T%d C' % (l, i))
        w, bw = ring_load(win[:, :, OFF_V:OFF_V + 512], (8, 512), cvin)
        cx.dma("sp", [(wf[:], win[:, :, OFF_F:OFF_F + 8])], b_wf, reads=[cvin], writes=[b_wf])
        for blk in range(4):
            ps, bps = pget("A")
            mm_group(ps[:], bps, [(hT[:, k, blk * 128:(blk + 1) * 128], w[:, k, 0:512]) for k in range(8)], [bw, b_hT])
            cx.op("act", lambda blk=blk, ps=ps: act.copy(out=Vc[:, i * 4 + blk, :, 0:64],
                                                          in_=ps[:].rearrange("p (h e) -> p h e", h=8)),
                  reads=[bps], writes=[b_Vc[i]])
        pf, bpf = pget("X")
        for blk in range(4):
            mm_group(pf[:, blk * 8:(blk + 1) * 8], bpf,
                     [(hT[:, k, blk * 128:(blk + 1) * 128], wf[:, k, :]) for k in range(8)], [b_wf, b_hT])
        cx.op("dve", lambda: dve.tensor_tensor(out=tf[:].rearrange("p (b e) -> p b e", b=4),
                                               in0=pf[:, 0:32].rearrange("p (b e) -> p b e", b=4),
                                               in1=bfB[:].unsqueeze(1).to_broadcast([128, 4, 8]), op=ALU.add),
              reads=[bpf, b_bfB], writes=[b_tf])
        cx.op("act", lambda: act.activation(out=tf[:], in_=tf[:], func=AF.Exp, scale=-1.0), reads=[b_tf], writes=[b_tf])
        cx.op("act", lambda: act.activation(out=nl[:], in_=tf[:], func=AF.Ln, bias=1.0, scale=1.0),
              reads=[b_tf], writes=[b_nl])
        pc, bpc = pget("X")
        for blk in range(4):
            prs_ = [(nl[:, b2 * 8:(b2 + 1) * 8], ones_f[:]) for b2 in range(blk)] + [(nl[:, blk * 8:(blk + 1) * 8], triU[:])]
            mm_group(pc[0:8, blk * 128:(blk + 1) * 128], bpc, prs_, [b_nl, b_const])
        cx.op("dve", lambda: dve.tensor_scalar(out=Cs[0:8, :], in0=pc[0:8, :], scalar1=carry[0:8, 0:1],
                                               scalar2=None, op0=ALU.add), reads=[bpc, b_carry], writes=[b_Cs])
        cx.op("act", lambda: act.copy(out=carry[0:8, 0:1], in_=Cs[0:8, TT - 1:TT]), reads=[b_Cs], writes=[b_carry])
        cx.op("dve", lambda: dve.tensor_copy(out=PIE[0:8, :], in_=Cs[0:8, :]), reads=[b_Cs], writes=[b_PIE])
        cx.op("dve", lambda: dve.tensor_tensor(out=r1[0:8, :], in0=Cs[0:8, :], in1=PIE[0:8, :], op=ALU.subtract),
              reads=[b_Cs, b_PIE], writes=[b_r1])
        cx.op("dve", lambda: dve.tensor_copy(out=lot[0:8, :], in_=r1[0:8, :]), reads=[b_r1], writes=[b_lot])
        cx.op("dve", lambda: dve.tensor_copy(out=PIE[32:40, :], in_=lot[0:8, :]), reads=[b_lot], writes=[b_PIE])
        cx.op("dve", lambda: dve.tensor_tensor(out=r1[0:8, :], in0=r1[0:8, :], in1=lot[0:8, :], op=ALU.subtract),
              reads=[b_r1, b_lot], writes=[b_r1])
        cx.op("dve", lambda: dve.tensor_copy(out=PIE[64:72, :], in_=r1[0:8, :]), reads=[b_r1], writes=[b_PIE])
        for h in range(8):
            for (Sel, dst_ap, bd) in ((SelK, Kt[h][64:70, t0:t0 + TT], b_Kt[h][i]), (SelQ, Qt[64:70, h, :], b_Qt[h])):
                pa, bpa = pget("S") if (h % 2 == 0) else pget("O")
                mm_group(pa[0:70, :], bpa, [(Sel[:, h, :], PIE[:])], [b_PIE, b_const])
                cx.op("act", lambda pa=pa, dst_ap=dst_ap: act.copy(out=dst_ap, in_=pa[64:70, :]), reads=[bpa], writes=[bd])
        MARK('L%d T%d D' % (l, i))

        def head_norm_g(ps, bps, ones_mat, inv_n, gcol_idx, out_ap, bd, n=TT):
            sq_, bsq = ef_next()
            cx.op("act", lambda: act.activation(out=sq_[:, 0:n], in_=ps[:, 0:n], func=AF.Square), reads=[bps], writes=[bsq])
            yield
            p2, bp2 = pget("EF")
            mm_group(p2[:, 0:n], bp2, [(ones_mat[:], sq_[:, 0:n])], [bsq, b_const])
            yield
            cx.op("act", lambda: act.activation(out=rs[:, 0:n], in_=p2[:, 0:n], func=AF.Sqrt, bias=epsc[:, 0:1], scale=inv_n),
                  reads=[bp2, b_const], writes=[b_rs])
            yield
            cx.op("dve", lambda: dve.reciprocal(out=rs[:, 0:n], in_=rs[:, 0:n]), reads=[b_rs], writes=[b_rs])
            yield
            cx.op("dve", lambda: dve.scalar_tensor_tensor(out=out_ap, in0=ps[:, 0:n], scalar=cols[:, gcol_idx:gcol_idx + 1],
                                                          in1=rs[:, 0:n], op0=ALU.mult, op1=ALU.mult),
                  reads=[bps, b_rs, b_cols], writes=[bd])
            yield

        def ef_steps():
            w, bw = ring_load(win[:, :, OFF_POOL:OFF_POOL + 512], (8, 512), cvin)
            yield
            for g in range(4):
                ps, bps = pget("EF")
                mm_group(ps[:], bps, [(w[:, k, g * 128:(g + 1) * 128], hT[:, k, 0:TT]) for k in range(8)], [bw, b_hT])
                yield
                cx.op("act", lambda ps=ps: act.copy(out=U[:, 16:16 + TT], in_=ps[:]), reads=[bps], writes=[b_U])
                cx.op("dve", lambda g=g: dve.tensor_copy(out=U[:, 0:16], in_=halo[:, g, :]), reads=[b_halo], writes=[b_U])
                yield
                cx.op("dve", lambda g=g: dve.tensor_copy(out=halo[:, g, :], in_=U[:, TT:TT + 16]), reads=[b_U], writes=[b_halo])
                E = 16 + TT
                srcL, bsrc = U, b_U
                lo = 0
                bufs = [(La, b_La), (Lb, b_Lb)]
                for lev in range(g + 1):
                    sh = 2 ** lev
                    dL, bdL = bufs[lev % 2]
                    lo2 = lo + sh
                    cx.op("pool", lambda dL=dL, srcL=srcL, lo2=lo2, sh=sh: pool.tensor_tensor(
                        out=dL[:, lo2:E], in0=srcL[:, lo2:E], in1=srcL[:, lo2 - sh:E - sh], op=ALU.add),
                        reads=[bsrc], writes=[bdL])
                    srcL, bsrc, lo = dL, bdL, lo2
                    yield
                wv_ = 2 ** (g + 1)
                dT, b_dT = ef_next()
                if i == 0:
                    cx.op("dve", lambda srcL=srcL, g=g: dve.tensor_tensor(out=srcL[:, 16:32], in0=srcL[:, 16:32],
                                                                        in1=invc[:, g, :], op=ALU.mult),
                          reads=[bsrc, b_const], writes=[bsrc])
                    yield
                cx.op("dve", lambda srcL=srcL, wv_=wv_, dT=dT: dve.scalar_tensor_tensor(
                    out=dT[:], in0=srcL[:, 16:E], scalar=1.0 / wv_, in1=U[:, 16:E], op0=ALU.mult, op1=ALU.subtract),
                    reads=[bsrc, b_U], writes=[b_dT])
                yield
                p2, bp2 = pget("EF")
                mm_group(p2[:], bp2, [(poolw[:, g, :], dT[:])], [b_poolw, b_dT])
                yield
                cx.op("act", lambda p2=p2, g=g: act.activation(out=brT[:, 0, g, :], in_=p2[:], func=AF.Identity,
                                                              scale=cols[:, 4 + g:5 + g]),
                      reads=[bp2, b_cols], writes=[b_brT[0]])
                yield
            w2, bw2 = ring_load(win[:, :, OFF_MQ:OFF_MQ + 512], (8, 512), cvin)
            yield
            for hm in range(4):
                ps, bps = pget("EF")
                mm_group(ps[:], bps, [(w2[:, k, hm * 128:(hm + 1) * 128], hT[:, k, 0:TT]) for k in range(8)], [bw2, b_hT])
                yield
                qm, b_qm = ef_next()
                yield from head_norm_g(ps, bps, ones_b, 1.0 / 128, 2, qm[:], b_qm)
                exs = []
                for mc in range(2):
                    s_, bs_ = pget("EF")
                    mm_group(s_[:], bs_, [(KmT[:, hm, mc * 128:(mc + 1) * 128], qm[:])], [b_KmT, b_qm])
                    yield
                    ex, bex = ef_next()
                    cx.op("act", lambda s_=s_, ex=ex: act.activation(out=ex[:], in_=s_[:], func=AF.Exp),
                          reads=[bs_], writes=[bex])
                    exs.append((ex, bex))
                    yield
                po, bpo = pget("EF")
                mm_group(po[:], bpo, [(Vm[:, mc, hm * 128:(hm + 1) * 128], exs[mc][0][:]) for mc in range(2)],
                         [b_Vm, exs[0][1], exs[1][1]])
                yield
                pq, bpq = pget("EF")
                mm_group(pq[:], bpq, [(ones_b[:], exs[mc][0][:]) for mc in range(2)], [b_const, exs[0][1], exs[1][1]])
                yield
                cx.op("dve", lambda pq=pq: dve.reciprocal(out=rec[:], in_=pq[:]), reads=[bpq], writes=[b_rec])
                yield
                cx.op("dve", lambda po=po, hm=hm: dve.tensor_tensor(out=brT[:, 2, hm, :], in0=po[:], in1=rec[:], op=ALU.mult),
                      reads=[bpo, b_rec], writes=[b_brT[2]])
                yield

        nkb = 4 * (i + 1)
        blocks = [(h, j) for h in range(8) for j in range(nkb)]
        NB_ = len(blocks)
        hstate = {}
        pend = {}
        fin_q = []

        def emit_S(n):
            h, j = blocks[n]
            jj = j - 4 * i
            q0 = max(0, jj) * 128
            ps, bps = pget("SA")

            def fs():
                ins = pe.matmul(ps[:, q0:TT], lhsT=Kt[h][0:70, j * 128:(j + 1) * 128], rhs=Qt[0:70, h, q0:TT],
                                start=True, stop=(jj < 0))
                if jj >= 0:
                    ins = pe.matmul(ps[:, q0:q0 + 128], lhsT=ident_b[:], rhs=maskT[:], start=False, stop=True)
                return ins
            cx.op("pe", fs, reads=[b_Kt[h][j // 4], b_Qt[h], b_const], writes=[bps])
            k3 = pt_next()
            cx.op("act", lambda: act.activation(out=pT[k3][:, q0:TT], in_=ps[:, q0:TT], func=AF.Exp),
                  reads=[bps], writes=[b_pT[k3]])
            pend[n] = (k3, q0)

        def emit_PV(n):
            h, j = blocks[n]
            k3, q0 = pend.pop(n)
            if j == 0:
                gb_ = gt_i[0] % 2
                gt_i[0] += 1
                hstate[h] = pget("O") + (gt[gb_], b_gt[gb_])
            po, bpo, gtt, bgtt = hstate[h]
            cx.op("pe", lambda: pe.matmul(po[0:65, q0:TT], lhsT=Vc[:, j, h, :], rhs=pT[k3][:, q0:TT],
                                          start=(j == 0), stop=(j == nkb - 1)),
                  reads=[b_Vc[j // 4], b_pT[k3]], writes=[bpo])
            if j == nkb - 1:
                cx.op("dve", lambda: dve.reciprocal(out=gtt[64:65, :], in_=po[64:65, :]), reads=[bpo], writes=[bgtt])
                fin_q.append((n + 4, h))

        def emit_fin(h):
            po, bpo, gtt, bgtt = hstate[h]
            pb_, bpb = pget("X")
            mm_group(pb_[0:64, :], bpb, [(ones_f[64:65, 0:64], gtt[64:65, :])], [bgtt, b_const])
            cx.op("dve", lambda: dve.tensor_copy(out=gtt[0:64, :], in_=pb_[0:64, :]), reads=[bpb], writes=[bgtt])
            hp = (h % 2) * 64
            cx.op("dve", lambda: dve.tensor_tensor(out=brT[hp:hp + 64, 1, h // 2, :], in0=po[0:64, :],
                                                   in1=gtt[0:64, :], op=ALU.mult),
                  reads=[bpo, bgtt], writes=[b_brT[1]])

        for e_ in ("dve", "act"):
            cx._deps(e_, [], [b_merged])
        gen = ef_steps()
        spb = max(1, -(-120 // NB_))
        LA_ = 2
        for n in range(NB_ + LA_):
            if n < NB_:
                emit_S(n)
            if n >= LA_:
                emit_PV(n - LA_)
            while fin_q and fin_q[0][0] <= n:
                emit_fin(fin_q.pop(0)[1])
            for _ in range(spb):
                next(gen, None)
        while fin_q:
            emit_fin(fin_q.pop(0)[1])
        for _ in gen:
            pass
        for e_ in ("dve", "act", "pe", "pool"):
            cx._deps(e_, [], b_efs)
        MARK('L%d T%d G' % (l, i))
        wbrs = (w_pool_br, w_fox_br, w_mem_br)
        for hc in range(4):
            for br in range(3):
                goff = OFF_G + br * 1024 + hc * 256
                wg, bwg = ring_load(win[:, :, goff:goff + 256], (8, 256), cvin)
                wbsrc, wbcv = CV["w_br%d_%d" % (br, l)]
                wb, bwb = ring_load(wbsrc.rearrange("(k p) n -> p k n", p=128)[:, :, hc * 256:(hc + 1) * 256], (4, 256), wbcv)
                for cc in range(2):
                    c = hc * 2 + cc
                    ps, bps = pget("A")
                    mm_group(ps[:], bps, [(wg[:, k, cc * 128:(cc + 1) * 128], hT[:, k, 0:TT]) for k in range(8)], [bwg, b_hT])
                    gi_ = gt_i[0] % 2
                    gt_i[0] += 1
                    cx.op("act", lambda ps=ps, gi_=gi_: act.activation(out=gt[gi_][:], in_=ps[:], func=AF.Sigmoid),
                          reads=[bps], writes=[b_gt[gi_]])
                    p2, bp2 = pget("S")
                    mm_group(p2[:], bp2, [(wb[:, k, cc * 128:(cc + 1) * 128], brT[:, br, k, :]) for k in range(4)],
                             [bwb, b_brT[br]])
                    if br == 0:
                        cx.op("dve", lambda cc=cc, gi_=gi_, p2=p2: dve.tensor_tensor(out=macc[:, cc, :], in0=gt[gi_][:],
                                                                                in1=p2[:], op=ALU.mult),
                              reads=[b_gt[gi_], bp2], writes=[b_macc[cc]])
                    else:
                        cx.op("dve", lambda gi_=gi_, p2=p2: dve.tensor_tensor(out=gt[gi_][:], in0=gt[gi_][:], in1=p2[:],
                                                                          op=ALU.mult),
                              reads=[b_gt[gi_], bp2], writes=[b_gt[gi_]])
                        if br == 1:
                            cx.op("pool", lambda cc=cc, gi_=gi_: pool.tensor_tensor(out=macc[:, cc, :], in0=macc[:, cc, :],
                                                                                in1=gt[gi_][:], op=ALU.add),
                                  reads=[b_gt[gi_], b_macc[cc]], writes=[b_macc[cc]])
                        else:
                            cx.op("pool", lambda cc=cc, gi_=gi_, c=c: pool.tensor_tensor(out=mergedT[:, c, :], in0=macc[:, cc, :],
                                                                                     in1=gt[gi_][:], op=ALU.add),
                                  reads=[b_gt[gi_], b_macc[cc]], writes=[b_merged])
        MARK('L%d T%d H' % (l, i))
        wo_v = CV["w_out%d" % l][0].rearrange("(k p) n -> p k n", p=128)
        wocv = CV["w_out%d" % l][1]
        for half in range(2):
            wo, bwo = ring_load(wo_v[:, :, half * 512:(half + 1) * 512], (8, 512), wocv)
            for blk in range(4):
                r0 = t0 + blk * 128
                xi = xs_i[0] % 2
                xs_i[0] += 1
                cx.dma("sp", [(xs[xi][:, 0:512], src[r0:r0 + 128, half * 512:(half + 1) * 512])], b_xs[xi], writes=[b_xs[xi]])
                ps, bps = pget("A")
                mm_group(ps[:], bps, [(mergedT[:, k, blk * 128:(blk + 1) * 128], wo[:, k, :]) for k in range(8)],
                         [bwo, b_merged])
                cx.op("dve", lambda xi=xi, ps=ps: dve.tensor_tensor(out=xs[xi][:, 0:512], in0=ps[:], in1=xs[xi][:, 0:512],
                                                                 op=ALU.add),
                      reads=[bps, b_xs[xi]], writes=[b_xs[xi]])
                cx.dma("sp", [(dst[r0:r0 + 128, half * 512:(half + 1) * 512], outs[xi][:, 0:512])], b_outs[xi],
                       reads=[b_outs[xi]], writes=[b_dram[id(dst)][(r0 // 128)]])

    b_dram = {id(scrA): [Buf("scrA%d" % i) for i in range(S // 128)],
              id(scrB): [Buf("scrB%d" % i) for i in range(S // 128)],
              id(out): [Buf("out%d" % i) for i in range(S // 128)],
              id(x_in): [Buf("xin%d" % i) for i in range(S // 128)]}

    def ffn_phase(l, src, dst, moe):
        nexp = NEXP if moe else 1
        F = DEXP if moe else DFF
        nf = F // 128
        if moe:
            cx.dma("sp", [(brB[:], b_router[0:1, :].to_broadcast([128, 8]))], b_brB, writes=[b_brB])
            cx.dma("pool", [(wr[:], w_router[0].rearrange("(k p) e -> p k e", p=128))], b_wr, writes=[b_wr])
        for t in range(S // FT):
            t0 = t * FT
            for blk in range(8):
                r0 = t0 + blk * 128
                cx._deps("sp", [b_dram[id(src)][r0 // 128]], [])
                norm_transpose(src[r0:r0 + 128, :], 2, hTh[blk // 4][:, :, (blk % 4) * 128:(blk % 4 + 1) * 128], b_hTh[blk // 4])
            if moe:
                pl, bpl = pget("X")
                for blk in range(8):
                    mm_group(pl[:, blk * 8:(blk + 1) * 8], bpl,
                             [(hTh[blk // 4][:, k, (blk % 4) * 128:(blk % 4 + 1) * 128], wr[:, k, :]) for k in range(8)],
                             [b_hT, b_QtAll, b_wr])
                v3 = lambda a: a.rearrange("p (b e) -> p b e", b=8)
                bro = lambda a: a.unsqueeze(2).to_broadcast([128, 8, 8])

                b_lg2, b_m1, b_m2 = Buf("lg2"), Buf("m1"), Buf("m2")
                cx.op("dve", lambda: dve.tensor_tensor(out=v3(lg[:]), in0=v3(pl[:, 0:64]),
                                                       in1=brB[:].unsqueeze(1).to_broadcast([128, 8, 8]), op=ALU.add),
                      reads=[bpl, b_brB], writes=[b_lg])
                cx.op("dve", lambda: dve.tensor_reduce(out=m1[:], in_=v3(lg[:]), axis=AX.X, op=ALU.max), reads=[b_lg], writes=[b_m1])
                cx.op("dve", lambda: dve.tensor_tensor(out=v3(lg2[:]), in0=v3(lg[:]), in1=bro(m1[:]), op=ALU.is_equal),
                      reads=[b_lg, b_m1], writes=[b_lg2])
                cx.op("dve", lambda: dve.scalar_tensor_tensor(out=lg2[:], in0=lg2[:], scalar=-1e30, in1=lg[:], op0=ALU.mult, op1=ALU.add),
                      reads=[b_lg, b_lg2], writes=[b_lg2])
                cx.op("dve", lambda: dve.tensor_reduce(out=m2[:], in_=v3(lg2[:]), axis=AX.X, op=ALU.max), reads=[b_lg2], writes=[b_m2])
                cx.op("dve", lambda: dve.tensor_tensor(out=v3(lg2[:]), in0=v3(lg[:]), in1=bro(m2[:]), op=ALU.is_ge),
                      reads=[b_lg, b_m2], writes=[b_lg2])
                cx.op("dve", lambda: dve.tensor_tensor(out=v3(lg[:]), in0=v3(lg[:]), in1=bro(m1[:]), op=ALU.subtract),
                      reads=[b_lg, b_m1], writes=[b_lg])
                cx.op("act", lambda: act.activation(out=lg[:], in_=lg[:], func=AF.Exp), reads=[b_lg], writes=[b_lg])
                cx.op("dve", lambda: dve.tensor_tensor(out=lg[:], in0=lg[:], in1=lg2[:], op=ALU.mult), reads=[b_lg, b_lg2], writes=[b_lg])
                cx.op("dve", lambda: dve.tensor_reduce(out=m1[:], in_=v3(lg[:]), axis=AX.X, op=ALU.add), reads=[b_lg], writes=[b_m1])
                cx.op("dve", lambda: dve.reciprocal(out=m1[:], in_=m1[:]), reads=[b_m1], writes=[b_m1])
                cx.op("dve", lambda: dve.tensor_tensor(out=v3(cw[:]), in0=v3(lg[:]), in1=bro(m1[:]), op=ALU.mult),
                      reads=[b_lg, b_m1], writes=[b_cw])
                pct, bpct = pget("S")
                pct2, bpct2 = pget("S")

                def ftr():
                    for blk in range(8):
                        tgt = pct if blk < 4 else pct2
                        ins = pe.transpose(tgt[0:8, (blk % 4) * 128:(blk % 4 + 1) * 128], cw[:, blk * 8:(blk + 1) * 8], ident_f[:])
                    return ins
                cx.op("pe", ftr, reads=[b_cw, b_const], writes=[bpct, bpct2])
                cx.op("act", lambda: act.copy(out=cwT[0:8, 0:512], in_=pct[0:8, :]), reads=[bpct], writes=[b_cwT])
                cx.op("act", lambda: act.copy(out=cwT[0:8, 512:1024], in_=pct2[0:8, :]), reads=[bpct2], writes=[b_cwT])
            for e in range(nexp):
                wgu = CV["w_ffn_gu"][0].rearrange("(k p) n -> p k n", p=128)
                wdn = CV["w_ffn_down"][0].rearrange("(f p) n -> p f n", p=128)
                cvgu, cvdn = CV["w_ffn_gu"][1], CV["w_ffn_down"][1]
                if moe:
                    ce = e % 2
                    cx.op("dve", lambda e=e: dve.tensor_scalar(out=cwTm[:], in0=cwT[:], scalar1=ident_f[0:8, e:e + 1], scalar2=None,
                                                              op0=ALU.mult), reads=[b_cwT, b_const], writes=[b_cwT])
                    for half in range(2):
                        pcb, bpcb = pget("X")
                        mm_group(pcb[:], bpcb, [(ones_f[0:8, :], cwTm[0:8, half * 512:(half + 1) * 512])], [b_cwT, b_const])
                        cx.op("act", lambda pcb=pcb, half=half, ce=ce: act.copy(out=cwB[ce][:, half * 512:(half + 1) * 512], in_=pcb[:]),
                              reads=[bpcb], writes=[b_cwB[ce]])
                f0 = 0
                while f0 < nf:
                    ng = min(4, nf - f0)
                    wg, bwg = ring_load(wgu[:, :, f0 * 128:(f0 + ng) * 128], (8, ng * 128), cvgu)
                    wu, bwu = ring_load(wgu[:, :, F + f0 * 128:F + (f0 + ng) * 128], (8, ng * 128), cvgu)
                    for fi in range(ng):
                        f = f0 + fi
                        for half in range(2):
                            hs = slice(half * 512, (half + 1) * 512)
                            pg, bpg = pget("A")
                            mm_group(pg[:], bpg, [(wg[:, k, fi * 128:(fi + 1) * 128], hTh[half][:, k, :]) for k in range(8)], [bwg, b_hTh[half]])
                            pu, bpu = pget("O")
                            mm_group(pu[:], bpu, [(wu[:, k, fi * 128:(fi + 1) * 128], hTh[half][:, k, :]) for k in range(8)], [bwu, b_hTh[half]])
                            gi_ = gt_i[0] % 2
                            gt_i[0] += 1
                            cx.op("act", lambda pg=pg, gi_=gi_: act.activation(out=gt[gi_][:], in_=pg[:], func=AF.Silu),
                                  reads=[bpg], writes=[b_gt[gi_]])
                            if moe:
                                cx.op("dve", lambda gi_=gi_, pu=pu: dve.tensor_tensor(out=gt[gi_][:], in0=gt[gi_][:], in1=pu[:], op=ALU.mult),
                                      reads=[b_gt[gi_], bpu], writes=[b_gt[gi_]])
                                cx.op("dve", lambda gi_=gi_, f=f, hs=hs, ce=ce: dve.tensor_tensor(out=aT[:, f, hs], in0=gt[gi_][:],
                                                                                           in1=cwB[ce][:, hs], op=ALU.mult),
                                      reads=[b_gt[gi_], b_cwB[ce]], writes=[b_cache])
                            else:
                                cx.op("dve", lambda gi_=gi_, pu=pu, f=f, hs=hs: dve.tensor_tensor(out=aT[:, f, hs], in0=gt[gi_][:],
                                                                                           in1=pu[:], op=ALU.mult),
                                      reads=[b_gt[gi_], bpu], writes=[b_cache])
                    f0 += ng
                for c in range(8):
                    wd, bwd = ring_load(wdn[:, :, c * 128:(c + 1) * 128], (nf, 128), cvdn)
                    for half in range(2):
                        hs = slice(half * 512, (half + 1) * 512)
                        py, bpy = pget("S")
                        mm_group(py[:], bpy, [(wd[:, f, :], aT[:, f, hs]) for f in range(nf)], [bwd, b_cache])
                        if e == 0:
                            cx.op("act", lambda py=py, c=c, hs=hs: act.copy(out=yT[:, c, hs], in_=py[:]), reads=[bpy], writes=[b_yT])
                        else:
                            cx.op("dve", lambda py=py, c=c, hs=hs: dve.tensor_tensor(out=yT[:, c, hs], in0=yT[:, c, hs], in1=py[:], op=ALU.add),
                                  reads=[bpy, b_yT], writes=[b_yT])
            for blk in range(8):
                r0 = t0 + blk * 128
                xi = xs_i[0] % 2
                xs_i[0] += 1
                cx.dma("sp", [(xs[xi][:], src[r0:r0 + 128, :])], b_xs[xi], reads=[b_dram[id(src)][r0 // 128]], writes=[b_xs[xi]])
                for half in range(2):
                    pt_, bpt = pget("A")

                    def ftr2(pt_=pt_, half=half, blk=blk):
                        for cc in range(4):
                            c = half * 4 + cc
                            ins = pe.transpose(pt_[:, cc * 128:(cc + 1) * 128], yT[:, c, blk * 128:(blk + 1) * 128], ident_f[:])
                        return ins
                    cx.op("pe", ftr2, reads=[b_yT, b_const], writes=[bpt])
                    cx.op("dve", lambda xi=xi, pt_=pt_, half=half: dve.tensor_tensor(
                        out=outs[xi][:, half * 512:(half + 1) * 512], in0=pt_[:], in1=xs[xi][:, half * 512:(half + 1) * 512], op=ALU.add),
                        reads=[bpt, b_xs[xi]], writes=[b_outs[xi]])
                cx.dma("sp", [(dst[r0:r0 + 128, :], outs[xi][:])], b_outs[xi], reads=[b_outs[xi]],
                       writes=[b_dram[id(dst)][r0 // 128]])

    def moe_routed(l, src, dst):
        F = DEXP
        nf = F // 128
        b_HgZ = b_HgZ0
        hg_ev = {}
        yg_ev = {}
        cx.dma("sp", [(brB[:], b_router[0:1, :].to_broadcast([128, 8]))], b_brB, writes=[b_brB])
        cx.dma("pool", [(wr[:], w_router[0].rearrange("(k p) e -> p k e", p=128))], b_wr, writes=[b_wr])
        cx.op("dve", lambda: dve.memset(carry_bc[:], 0.0), writes=[b_cbc])
        lg, lgm = rt[:, 0:8], rt[:, 8:16]
        E2 = rt[:, 16:32].rearrange("p (k e) -> p k e", k=2)
        sel, pos, slotf, valid = rt[:, 32:40], rt[:, 40:48], rt[:, 48:56], rt[:, 56:64]
        prod = rt[:, 64:80].rearrange("p (k e) -> p k e", k=2)
        m1, m2, dd, e2, den = rt[:, 80:81], rt[:, 81:82], rt[:, 82:83], rt[:, 83:84], rt[:, 84:85]
        w12, s12, v12, t12 = rt[:, 85:87], rt[:, 87:89], rt[:, 89:91], rt[:, 91:93]
        R = [b_rt]

        def D_(fn, reads=(), writes=()):
            cx.op("dve", fn, reads=list(reads) + R, writes=list(writes) + R)

        for bi in range(S // 128):
            r0 = bi * 128
            i = xs_i[0] % 2
            xs_i[0] += 1
            xt, bx = xs[i], b_xs[i]
            hb, b_hb = hbs[i], b_hbs[i]
            cx.dma("sp", [(xt[:], src[r0:r0 + 128, :])], bx, reads=[b_dram[id(src)][bi]], writes=[bx])
            cx.op("act", lambda: act.activation(out=hb[:], in_=xt[:], func=AF.Square, accum_out=ss1[:, 0:1]),
                  reads=[bx], writes=[b_hb, b_ss1])
            cx.op("act", lambda: act.activation(out=ss1[:, 1:2], in_=ss1[:, 0:1], func=AF.Sqrt, bias=epsc[:, 0:1],
                                                scale=1.0 / D), reads=[b_ss1, b_const], writes=[b_ss1])
            cx.op("dve", lambda: dve.reciprocal(out=ss1[:, 1:2], in_=ss1[:, 1:2]), reads=[b_ss1], writes=[b_ss1])
            cx.op("act", lambda: act.activation(out=hb[:], in_=xt[:], func=AF.Identity, scale=ss1[:, 1:2]),
                  reads=[bx, b_ss1], writes=[b_hb])
            pt, bp = pget("X")
            ptb = pt.bitcast(BF16)

            def ftp():
                for k in range(8):
                    ins = pe.transpose(ptb[:, k * 128:(k + 1) * 128], hb[:, k * 128:(k + 1) * 128], ident_b[:])
                return ins
            cx.op("pe", ftp, reads=[b_hb, b_const], writes=[bp])
            cx.op("dve", lambda: dve.tensor_tensor(out=hT[:, :, 0:128], in0=ptb[:, 0:1024].rearrange("p (k t) -> p k t", k=8),
                                                   in1=gcols[:, 2, :].unsqueeze(2).to_broadcast([128, 8, 128]), op=ALU.mult),
                  reads=[bp, b_gcols], writes=[b_hT])
            pl, bpl = pget("O")
            mm_group(pl[:, 0:8], bpl, [(hT[:, k, 0:128], wr[:, k, :]) for k in range(8)], [b_hT, b_wr])
            D_(lambda: dve.tensor_tensor(out=lg, in0=pl[:, 0:8], in1=brB[:], op=ALU.add), reads=[bpl, b_brB])
            D_(lambda: dve.tensor_reduce(out=m1, in_=lg, axis=AX.X, op=ALU.max))
            D_(lambda: dve.tensor_tensor(out=E2[:, 0, :], in0=lg, in1=m1.to_broadcast([128, 8]), op=ALU.is_equal))
            D_(lambda: dve.scalar_tensor_tensor(out=lgm, in0=E2[:, 0, :], scalar=-1e30, in1=lg, op0=ALU.mult, op1=ALU.add))
            D_(lambda: dve.tensor_reduce(out=m2, in_=lgm, axis=AX.X, op=ALU.max))
            D_(lambda: dve.tensor_tensor(out=E2[:, 1, :], in0=lgm, in1=m2.to_broadcast([128, 8]), op=ALU.is_equal))
            D_(lambda: dve.tensor_tensor(out=sel, in0=E2[:, 0, :], in1=E2[:, 1, :], op=ALU.add))
            D_(lambda: dve.tensor_tensor(out=dd, in0=m2, in1=m1, op=ALU.subtract))
            cx.op("act", lambda: act.activation(out=e2, in_=dd, func=AF.Exp), reads=R, writes=R)
            D_(lambda: dve.tensor_scalar(out=den, in0=e2, scalar1=1.0, scalar2=None, op0=ALU.add))
            D_(lambda: dve.reciprocal(out=w12[:, 0:1], in_=den))
            D_(lambda: dve.tensor_tensor(out=w12[:, 1:2], in0=e2, in1=w12[:, 0:1], op=ALU.mult))
            pcs, bpcs = pget("S")
            mm_group(pcs[:, 0:8], bpcs, [(triU[:], sel)], R + [b_const])
            ptot, bptot = pget("S")
            mm_group(ptot[:, 0:8], bptot, [(ones_f[:], sel)], R + [b_const])
            D_(lambda: dve.tensor_tensor(out=pos, in0=pcs[:, 0:8], in1=carry_bc[:], op=ALU.add), reads=[bpcs, b_cbc])
            cx.op("dve", lambda: dve.tensor_tensor(out=carry_bc[:], in0=carry_bc[:], in1=ptot[:, 0:8], op=ALU.add),
                  reads=[bptot, b_cbc] + R, writes=[b_cbc])
            D_(lambda: dve.tensor_scalar(out=valid, in0=pos, scalar1=float(CAP), scalar2=None, op0=ALU.is_le))
            D_(lambda: dve.tensor_tensor(out=slotf, in0=pos, in1=eCm1[:], op=ALU.add), reads=[b_const])
            D_(lambda: dve.tensor_tensor(out=prod, in0=E2, in1=slotf.unsqueeze(1).to_broadcast([128, 2, 8]), op=ALU.mult))
            D_(lambda: dve.tensor_reduce(out=s12, in_=prod, axis=AX.X, op=ALU.add))
            D_(lambda: dve.tensor_tensor(out=prod, in0=E2, in1=valid.unsqueeze(1).to_broadcast([128, 2, 8]), op=ALU.mult))
            D_(lambda: dve.tensor_reduce(out=v12, in_=prod, axis=AX.X, op=ALU.add))
            D_(lambda: dve.tensor_tensor(out=t12, in0=s12, in1=v12, op=ALU.mult))
            D_(lambda: dve.tensor_copy(out=gidx[:, bi, :], in_=t12), writes=[b_gidx])
            D_(lambda: dve.tensor_tensor(out=wts[:, bi, :], in0=w12, in1=v12, op=ALU.mult), writes=[b_wts])
            D_(lambda: dve.scalar_tensor_tensor(out=t12, in0=s12, scalar=-BIG, in1=v12, op0=ALU.add, op1=ALU.mult))
            D_(lambda: dve.tensor_scalar(out=t12, in0=t12, scalar1=BIG, scalar2=None, op0=ALU.add))
            si_, bsi = sidx[bi % 2], b_sidx[bi % 2]
            D_(lambda: dve.tensor_copy(out=si_[:], in_=t12), writes=[bsi])
            cx._wait("pool", b_HgZ.writer)
            for k in range(2):
                ev = cx.dma("pool", None, b_hb, reads=[b_hb, bsi], writes=[Buf("hgw")],
                            issue=lambda k=k: pool.indirect_dma_start(
                                out=Hg[:, :], out_offset=bass.IndirectOffsetOnAxis(ap=si_[:, k:k + 1], axis=0),
                                in_=hb[:, :], in_offset=None, bounds_check=bcreg, oob_is_err=False))
                hg_ev[ev[1]] = ev
        MARK('MOE experts')
        segT = [(hT, b_hT), (Qt, b_QtAll), (mergedT, b_merged)]
        for e in range(NEXP):
            wgu = w_exp_gu[0, e].rearrange("(k p) n -> p k n", p=128)
            wdn = w_exp_down[0, e].rearrange("(f p) n -> p f n", p=128)
            MARK('MOE e%d load' % e)
            for b in range(CAP // 128):
                i = xs_i[0] % 2
                xs_i[0] += 1
                hb, b_hb = hbs[i], b_hbs[i]
                for ev in hg_ev.values():
                    cx._wait("sp", ev)
                rr = e * CAP + b * 128
                cx.dma("sp", [(hb[:], Hg[rr:rr + 128, :])], b_hb, writes=[b_hb])
                pt, bp = pget("X")
                ptb = pt.bitcast(BF16)

                def ftp2(hb=hb, ptb=ptb):
                    for k in range(8):
                        ins = pe.transpose(ptb[:, k * 128:(k + 1) * 128], hb[:, k * 128:(k + 1) * 128], ident_b[:])
                    return ins
                cx.op("pe", ftp2, reads=[b_hb, b_const], writes=[bp])
                st, bst = segT[b // 4]
                c0 = (b % 4) * 128
                cx.op("dve", lambda st=st, c0=c0, ptb=ptb: dve.tensor_tensor(
                    out=st[:, :, c0:c0 + 128], in0=ptb[:, 0:1024].rearrange("p (k t) -> p k t", k=8),
                    in1=gcols[:, 2, :].unsqueeze(2).to_broadcast([128, 8, 128]), op=ALU.mult),
                    reads=[bp, b_gcols], writes=[bst])
            MARK('MOE e%d gu' % e)
            f0 = 0
            while f0 < nf:
                ng = min(4, nf - f0)
                wg, bwg = ring_load(wgu[:, :, f0 * 128:(f0 + ng) * 128], (8, ng * 128))
                wu, bwu = ring_load(wgu[:, :, F + f0 * 128:F + (f0 + ng) * 128], (8, ng * 128))
                for fi in range(ng):
                    f = f0 + fi
                    for si2, (s0, n) in enumerate(SEGS):
                        st, bst = segT[si2]
                        pg, bpg = pget("A")
                        mm_group(pg[:, 0:n], bpg, [(wg[:, k, fi * 128:(fi + 1) * 128], st[:, k, 0:n]) for k in range(8)], [bwg, bst])
                        pu, bpu = pget("O")
                        mm_group(pu[:, 0:n], bpu, [(wu[:, k, fi * 128:(fi + 1) * 128], st[:, k, 0:n]) for k in range(8)], [bwu, bst])
                        gi_ = gt_i[0] % 2
                        gt_i[0] += 1
                        cx.op("act", lambda pg=pg, gi_=gi_, n=n: act.activation(out=gt[gi_][:, 0:n], in_=pg[:, 0:n], func=AF.Silu),
                              reads=[bpg], writes=[b_gt[gi_]])
                        cx.op("dve", lambda gi_=gi_, pu=pu, f=f, s0=s0, n=n: dve.tensor_tensor(
                            out=aTr[:, f, s0:s0 + n], in0=gt[gi_][:, 0:n], in1=pu[:, 0:n], op=ALU.mult),
                            reads=[b_gt[gi_], bpu], writes=[b_cache])
                f0 += ng
            MARK('MOE e%d down' % e)
            for chalf in range(2):
                for cc in range(4):
                    c = chalf * 4 + cc
                    wd, bwd = ring_load(wdn[:, :, c * 128:(c + 1) * 128], (nf, 128))
                    for (s0, n) in SEGS:
                        py, bpy = pget("S")
                        mm_group(py[:, 0:n], bpy, [(wd[:, f, :], aTr[:, f, s0:s0 + n]) for f in range(nf)], [bwd, b_cache])
                        cx.op("act", lambda py=py, cc=cc, s0=s0, n=n: act.copy(out=yTs[:, cc, s0:s0 + n], in_=py[:, 0:n]),
                              reads=[bpy], writes=[b_yT])
                for b in range(CAP // 128):
                    i = xs_i[0] % 2
                    xs_i[0] += 1
                    pt_, bpt = pget("A")

                    def ftr3(pt_=pt_, b=b):
                        for cc in range(4):
                            ins = pe.transpose(pt_[:, cc * 128:(cc + 1) * 128], yTs[:, cc, b * 128:(b + 1) * 128], ident_f[:])
                        return ins
                    cx.op("pe", ftr3, reads=[b_yT, b_const], writes=[bpt])
                    cx.op("dve", lambda i=i, pt_=pt_: dve.tensor_copy(out=xs[i][:, 0:512], in_=pt_[:]), reads=[bpt], writes=[b_xs[i]])
                    rr = e * CAP + b * 128
                    ev = cx.dma("sp", [(Yg[rr:rr + 128, chalf * 512:(chalf + 1) * 512], xs[i][:, 0:512])], b_xs[i],
                                reads=[b_xs[i]], writes=[Buf("ygw")])
                    yg_ev[ev[1]] = ev
        MARK('MOE combine')
        ybuf = [brT[:, k, :, :].rearrange("p a t -> p (a t)").bitcast(F32) for k in range(2)]
        for bi in range(S // 128):
            r0 = bi * 128
            i = xs_i[0] % 2
            xs_i[0] += 1
            cx.dma("sp", [(xs[i][:], src[r0:r0 + 128, :])], b_xs[i], reads=[b_dram[id(src)][bi]], writes=[b_xs[i]])
            for k in range(2):
                for ev in yg_ev.values():
                    cx._wait("pool", ev)
                bb = b_brT[k]
                cx.dma("pool", None, bb, reads=[b_gidx], writes=[bb],
                       issue=lambda k=k, bi=bi: pool.indirect_dma_start(
                           out=ybuf[k][:, :], out_offset=None, in_=Yg[:, :],
                           in_offset=bass.IndirectOffsetOnAxis(ap=gidx[:, bi, k:k + 1], axis=0),
                           bounds_check=bcreg, oob_is_err=False))
                cx.op("dve", lambda i=i, k=k, bi=bi: dve.scalar_tensor_tensor(
                    out=xs[i][:], in0=ybuf[k][:], scalar=wts[:, bi, k:k + 1], in1=xs[i][:], op0=ALU.mult, op1=ALU.add),
                    reads=[bb, b_wts, b_xs[i]], writes=[b_xs[i]])
            cx.dma("sp", [(dst[r0:r0 + 128, :], xs[i][:])], b_xs[i], reads=[b_xs[i]], writes=[b_dram[id(dst)][bi]])

    b_yT = Buf("yT")

    def phase_barrier(to_ffn):
        allb = [b for hb_ in b_Kt for b in hb_] + b_Vc
        if to_ffn:
            for e in ("dve", "act", "pe"):
                cx._deps(e, [], allb)
        else:
            for e in ("dve", "act", "pe", "pool"):
                cx._deps(e, [], [b_cache, b_yT])

    for l_ in range(DEPTH):
        convert("w_mem_kv%d" % l_, w_mem_kv[l_])
        convert("w_in%d" % l_, w_in[l_])
        for br_, wsrc_ in enumerate((w_pool_br, w_fox_br, w_mem_br)):
            convert("w_br%d_%d" % (br_, l_), wsrc_[l_])
        convert("w_out%d" % l_, w_out[l_])
        if l_ == 0:
            convert("w_ffn_gu", w_ffn_gu[0])
            convert("w_ffn_down", w_ffn_down[0])
    b_HgZ0 = Buf("HgZ0")
    cx.dma("sp", [(Hg[bz * 128:(bz + 1) * 128, :], hbs[0][:]) for bz in range(NSL // 128)], b_hbs[0],
           reads=[b_hbs[0]], writes=[b_HgZ0])
    chain = [(x_in, scrA), (scrA, scrB), (scrB, scrA), (scrA, out)]
    for l in range(dbg.get("layers", DEPTH)):
        load_layer_consts(l)
        mem_kv(l)
        src, dst = chain[2 * l]
        if l > 0:
            cx.op("pool", lambda: pool.memset(halo[:], 0.0), writes=[b_halo])
            cx.op("pool", lambda: pool.memset(carry[:], 0.0), writes=[b_carry])
            cx.op("pool", lambda: pool.memset(Vc[:, :, :, 64:65], 1.0), writes=b_Vc)
        for i in range(dbg.get("tiles", NT)):
            for blk in range(4):
                cx._deps("sp", [b_dram[id(src)][(i * TT) // 128 + blk]], [])
            mixer_tile(l, i, src, dst)
        if dbg.get("dump_mixer") and l == dbg.get("layers", DEPTH) - 1:
            break
        MARK('L%d FFN' % l)
        phase_barrier(True)
        src, dst = chain[2 * l + 1]
        if l % 2 == 1:
            moe_routed(l, src, dst)
        else:
            ffn_phase(l, src, dst, moe=False)
        phase_barrier(False)
    if dbg.get("dump_mixer"):
        dumps = {"d_hT": (hT, [b_hT]), "d_Qt": (Qt, [b_QtAll]), "d_cache": (cache, [b for hb_ in b_Kt for b in hb_] + b_Vc),
                 "d_brT": (brT, b_brT), "d_merged": (mergedT, [b_merged]), "d_Cs": (Cs, [b_Cs]), "d_PIE": (PIE, [b_PIE]),
                 "d_KmT": (KmT, [b_KmT]), "d_Vm": (Vm, [b_Vm]), "d_cols": (cols, [b_cols]), "d_nl": (nl, [b_nl]),
                 "d_invc": (invc, [b_const]), "d_tmpf": (tmpf, [b_tmpf]), "d_U": (U, [b_U]),
                 "d_pT0": (pT[0], [b_pT[0]]), "d_pT1": (pT[1], [b_pT[1]]), "d_pT2": (pT[2], [b_pT[2]]), "d_pT3": (pT[3], [b_pT[3]]),
                 "d_La": (La, [b_La]), "d_Lb": (Lb, [b_Lb])}
        if dbg.get("nodump"):
            dumps = {}
        for nm, (ap_, bufs) in dumps.items():
            shp = list(ap_.shape)
            dt_ = ap_.dtype
            o_ = nc.dram_tensor(nm, shp, dt_, kind="ExternalOutput").ap()
            cx.dma("sp", [(o_, ap_)], bufs[0], reads=bufs, writes=[Buf("dd_" + nm)])
            cx.finish([bufs[0]])
        for b_ in b_dram[id(scrA)] + b_dram[id(scrB)]:
            cx.finish([b_])
    cx.finish(b_dram[id(out)])
    MARK('END')
    nc._marks = MARKS
    return nc


_NC_CACHE = {}


def kernel(**inputs):
    if "nc" not in _NC_CACHE:
        _NC_CACHE["nc"] = build_program()
    nc = _NC_CACHE["nc"]
    names = ["mix_norm_g", "w_in", "b_forget", "fox_q_g", "fox_k_g", "pool_w", "pool_scale", "mem_norm_g",
             "w_mem_kv", "mem_q_g", "mem_k_g", "w_pool_br", "w_fox_br", "w_mem_br", "w_out", "ffn_norm_g",
             "w_ffn_gu", "w_ffn_down", "w_router", "b_router", "w_exp_gu", "w_exp_down"]
    shared = {n: np.ascontiguousarray(np.asarray(inputs[n], dtype=np.float32)) for n in names}
    x = np.asarray(inputs["x"], dtype=np.float32)
    mem = np.asarray(inputs["mem"], dtype=np.float32)
    in_maps = []
    for b in range(8):
        m = dict(shared)
        m["x"] = np.ascontiguousarray(x[b])
        m["mem"] = np.ascontiguousarray(mem[b])
        in_maps.append(m)
    res = run_bass_kernel_spmd(nc, in_maps, core_ids=list(range(8)))
    return np.stack([np.asarray(res.results[b]["out"]) for b in range(8)], axis=0).astype(np.float32)
```
